# Optimizing a Trainium2 kernel written in Bass

```python
import math
import jax, jax.numpy as jnp
from jax import lax
import numpy as np

D_MODEL = 2048
BATCH = 1
SEQ = 8192
DEPTH = 1

CHUNK = 64
Q_BLOCK = 128
ROPE_THETA = 500000.0
EPS = 1e-6

MLA_HEADS = 8
MLA_Q_RANK = 768
MLA_KV_RANK = 512
MLA_NOPE = 128
MLA_ROPE = 64
MLA_V = 128

DIFF_HEADS = 8
DIFF_HEAD_DIM = 64
DIFF_ROT = DIFF_HEAD_DIM // 4
DIFF_QK = DIFF_HEADS * 2 * DIFF_HEAD_DIM
DIFF_VW = DIFF_HEADS * 2 * DIFF_HEAD_DIM

D_MIX = MLA_HEADS * MLA_V + DIFF_VW
IN_SPLITS = (MLA_Q_RANK, MLA_KV_RANK, MLA_ROPE, DIFF_QK, DIFF_QK, DIFF_VW)
D_IN = MLA_Q_RANK + MLA_KV_RANK + MLA_ROPE + 2 * DIFF_QK + DIFF_VW

N_EXPERTS = 64
TOP_K = 8
N_GROUPS = 8
TOPK_GROUPS = 4
D_EXPERT = 512
D_SHARED = 512
ROUTED_SCALE = 2.5
MOE_BLOCK = 128

kernel_name = "hybrid_mla_diffattn_moe_block"


def rmsnorm(x, g):
    xf = x.astype(jnp.float32)
    y = xf * lax.rsqrt(jnp.mean(xf * xf, axis=-1, keepdims=True) + EPS)
    return y.astype(x.dtype) * g


def rope_tables(seq, dim, dtype):
    pos = jnp.arange(seq, dtype=jnp.float32)
    inv = ROPE_THETA ** (-jnp.arange(0, dim, 2, dtype=jnp.float32) / dim)
    ang = pos[:, None] * inv[None, :]
    return jnp.cos(ang).astype(dtype), jnp.sin(ang).astype(dtype)


def rotate(x, cos, sin):
    half = x.shape[-1] // 2
    x1, x2 = x[..., :half], x[..., half:]
    return jnp.concatenate([x1 * cos - x2 * sin, x2 * cos + x1 * sin], axis=-1)


def to_blocks(a):
    b, s = a.shape[:2]
    nb = s // Q_BLOCK
    return jnp.moveaxis(a.reshape((b, nb, Q_BLOCK) + a.shape[2:]), 1, 0)


def from_blocks(a):
    a = jnp.moveaxis(a, 0, 1)
    return a.reshape((a.shape[0], a.shape[1] * a.shape[2]) + a.shape[3:])


def chunk_mask(start, seq):
    qpos = start + jnp.arange(Q_BLOCK, dtype=jnp.int32)
    kpos = jnp.arange(seq, dtype=jnp.int32)
    return (kpos[None, :] // CHUNK) <= (qpos[:, None] // CHUNK)


def sweep_query_blocks(fn, q_arrays):
    seq = q_arrays[0].shape[1]
    nb = seq // Q_BLOCK
    blocks = tuple(to_blocks(a) for a in q_arrays)
    starts = jnp.arange(nb, dtype=jnp.int32) * Q_BLOCK
    return from_blocks(lax.map(fn, blocks + (starts,)))


def mla_mixer(q_lat, kv_lat, k_pe, g_q, w_uq, g_kv, w_ukv):
    b, s, _ = q_lat.shape
    cos, sin = rope_tables(s, MLA_ROPE, q_lat.dtype)
    q = (rmsnorm(q_lat, g_q) @ w_uq).reshape(b, s, MLA_HEADS, MLA_NOPE + MLA_ROPE)
    q_nope = q[..., :MLA_NOPE]
    q_pe = rotate(q[..., MLA_NOPE:], cos[None, :, None, :], sin[None, :, None, :])
    kv = (rmsnorm(kv_lat, g_kv) @ w_ukv).reshape(b, s, MLA_HEADS, MLA_NOPE + MLA_V)
    k_nope, v = kv[..., :MLA_NOPE], kv[..., MLA_NOPE:]
    k_pe = rotate(k_pe, cos[None], sin[None])
    scale = (MLA_NOPE + MLA_ROPE) ** -0.5

    def block(args):
        qn, qp, start = args
        sc = (jnp.einsum('bqhd,bkhd->bhqk', qn, k_nope)
              + jnp.einsum('bqhd,bkd->bhqk', qp, k_pe)).astype(jnp.float32) * scale
        sc = jnp.where(chunk_mask(start, s)[None, None], sc, -jnp.inf)
        p = jax.nn.softmax(sc, axis=-1).astype(v.dtype)
        return jnp.einsum('bhqk,bkhd->bqhd', p, v)

    out = sweep_query_blocks(block, (q_nope, q_pe))
    return out.reshape(b, s, MLA_HEADS * MLA_V)


def diff_mixer(q, k, v, lq1, lk1, lq2, lk2, g_sub, lambda_init):
    b, s, _ = q.shape
    cos, sin = rope_tables(s, DIFF_ROT, q.dtype)
    cb, sb = cos[None, :, None, None, :], sin[None, :, None, None, :]
    q = q.reshape(b, s, DIFF_HEADS, 2, DIFF_HEAD_DIM)
    k = k.reshape(b, s, DIFF_HEADS, 2, DIFF_HEAD_DIM)
    v = v.reshape(b, s, DIFF_HEADS, 2 * DIFF_HEAD_DIM)
    q = jnp.concatenate([rotate(q[..., :DIFF_ROT], cb, sb), q[..., DIFF_ROT:]], axis=-1)
    k = jnp.concatenate([rotate(k[..., :DIFF_ROT], cb, sb), k[..., DIFF_ROT:]], axis=-1)
    lam = (jnp.exp(jnp.sum(lq1.astype(jnp.float32) * lk1.astype(jnp.float32)))
           - jnp.exp(jnp.sum(lq2.astype(jnp.float32) * lk2.astype(jnp.float32)))
           + lambda_init)
    scale = DIFF_HEAD_DIM ** -0.5

    def block(args):
        qb, start = args
        sc = jnp.einsum('bqhcd,bkhcd->cbhqk', qb, k).astype(jnp.float32) * scale
        sc = jnp.where(chunk_mask(start, s)[None, None, None], sc, -jnp.inf)
        p = jax.nn.softmax(sc, axis=-1)
        a = (p[0] - lam * p[1]).astype(v.dtype)
        return jnp.einsum('bhqk,bkhe->bqhe', a, v)

    out = sweep_query_blocks(block, (q,))
    out = rmsnorm(out, g_sub) * (1.0 - lambda_init)
    return out.reshape(b, s, DIFF_VW)


def swiglu(h, w_gate, w_up, w_down):
    return (jax.nn.silu(h @ w_gate) * (h @ w_up)) @ w_down


def moe(h, w_router, b_router, w_gate, w_up, w_down, ws_gate, ws_up, ws_down):
    b, s, d = h.shape
    t = b * s
    hf = h.reshape(t, d)
    scores = jax.nn.sigmoid(hf.astype(jnp.float32) @ w_router.astype(jnp.float32))
    biased = scores + b_router.astype(jnp.float32)
    grouped = biased.reshape(t, N_GROUPS, N_EXPERTS // N_GROUPS)
    gscore = jnp.sum(lax.top_k(grouped, 2)[0], axis=-1)
    _, gidx = lax.top_k(gscore, TOPK_GROUPS)
    gmask = jnp.zeros((t, N_GROUPS), dtype=bool).at[jnp.arange(t)[:, None], gidx].set(True)
    emask = jnp.repeat(gmask, N_EXPERTS // N_GROUPS, axis=1)
    _, eidx = lax.top_k(jnp.where(emask, biased, -jnp.inf), TOP_K)
    wts = jnp.take_along_axis(scores, eidx, axis=1)
    wts = wts / jnp.sum(wts, axis=-1, keepdims=True) * ROUTED_SCALE

    tk = t * TOP_K
    e_flat = eidx.reshape(tk).astype(jnp.int32)
    tok_flat = jnp.repeat(jnp.arange(t, dtype=jnp.int32), TOP_K)
    w_flat = wts.reshape(tk)
    order = jnp.argsort(e_flat, stable=True)
    se = e_flat[order]
    counts = jnp.bincount(e_flat, length=N_EXPERTS).astype(jnp.int32)
    padded = (counts + MOE_BLOCK - 1) // MOE_BLOCK * MOE_BLOCK
    pend = jnp.cumsum(padded)
    pstart = pend - padded
    gstart = jnp.cumsum(counts) - counts
    dest = pstart[se] + jnp.arange(tk, dtype=jnp.int32) - gstart[se]
    m_pad = tk + N_EXPERTS * MOE_BLOCK
    n_blk = m_pad // MOE_BLOCK
    row_tok = jnp.full((m_pad,), t, dtype=jnp.int32).at[dest].set(tok_flat[order])
    row_w = jnp.zeros((m_pad,), dtype=jnp.float32).at[dest].set(w_flat[order])
    blk_start = jnp.arange(n_blk, dtype=jnp.int32) * MOE_BLOCK
    blk_e = jnp.minimum(jnp.searchsorted(pend, blk_start, side='right'), N_EXPERTS - 1)
    h_pad = jnp.concatenate([hf, jnp.zeros((1, d), dtype=hf.dtype)], axis=0)

    def expert_block(args):
        rows, rw, e = args
        y = swiglu(h_pad[rows], w_gate[e], w_up[e], w_down[e])
        return y * rw[:, None].astype(y.dtype)

    ys = lax.map(expert_block, (row_tok.reshape(n_blk, MOE_BLOCK),
                                row_w.reshape(n_blk, MOE_BLOCK), blk_e))
    routed = jax.ops.segment_sum(ys.reshape(m_pad, d), row_tok, num_segments=t + 1)[:t]
    shared = swiglu(hf, ws_gate, ws_up, ws_down)
    return (shared + routed).reshape(b, s, d)


def setup_inputs(seed: int = 0) -> dict:
    key = jax.random.key(seed)
    ks = jax.random.split(key, 32)
    f32 = jnp.float32
    L, D = DEPTH, D_MODEL

    def nrm(k, shape, scale):
        return jax.random.normal(k, shape, dtype=f32) * scale

    def gain(k, shape):
        return 1.0 + 0.02 * jax.random.normal(k, shape, dtype=f32)

    return {
        "x": nrm(ks[0], (BATCH, SEQ, D), 1.0),
        "c": nrm(ks[1], (BATCH, D), 1.0),
        "w_ada": nrm(ks[2], (L, D, 6 * D), 0.5 * D ** -0.5),
        "b_ada": nrm(ks[3], (L, 6 * D), 0.02),
        "g_pre_mix": gain(ks[4], (L, D)),
        "w_in": nrm(ks[5], (L, D, D_IN), D ** -0.5),
        "g_q_lat": gain(ks[6], (L, MLA_Q_RANK)),
        "w_uq": nrm(ks[7], (L, MLA_Q_RANK, MLA_HEADS * (MLA_NOPE + MLA_ROPE)), MLA_Q_RANK ** -0.5),
        "g_kv_lat": gain(ks[8], (L, MLA_KV_RANK)),
        "w_ukv": nrm(ks[9], (L, MLA_KV_RANK, MLA_HEADS * (MLA_NOPE + MLA_V)), MLA_KV_RANK ** -0.5),
        "lambda_q1": nrm(ks[10], (L, DIFF_HEAD_DIM), 0.1),
        "lambda_k1": nrm(ks[11], (L, DIFF_HEAD_DIM), 0.1),
        "lambda_q2": nrm(ks[12], (L, DIFF_HEAD_DIM), 0.1),
        "lambda_k2": nrm(ks[13], (L, DIFF_HEAD_DIM), 0.1),
        "g_diff_sub": gain(ks[14], (L, 2 * DIFF_HEAD_DIM)),
        "w_out": nrm(ks[15], (L, D_MIX, D), D_MIX ** -0.5),
        "g_post_mix": gain(ks[16], (L, D)),
        "g_pre_ffn": gain(ks[17], (L, D)),
        "w_router": nrm(ks[18], (L, D, N_EXPERTS), D ** -0.5),
        "b_router": nrm(ks[19], (L, N_EXPERTS), 0.01),
        "w_gate": nrm(ks[20], (L, N_EXPERTS, D, D_EXPERT), D ** -0.5),
        "w_up": nrm(ks[21], (L, N_EXPERTS, D, D_EXPERT), D ** -0.5),
        "w_down": nrm(ks[22], (L, N_EXPERTS, D_EXPERT, D), D_EXPERT ** -0.5),
        "ws_gate": nrm(ks[23], (L, D, D_SHARED), D ** -0.5),
        "ws_up": nrm(ks[24], (L, D, D_SHARED), D ** -0.5),
        "ws_down": nrm(ks[25], (L, D_SHARED, D), D_SHARED ** -0.5),
        "g_post_ffn": gain(ks[26], (L, D)),
    }


def reference(x, c, w_ada, b_ada, g_pre_mix, w_in, g_q_lat, w_uq, g_kv_lat, w_ukv,
              lambda_q1, lambda_k1, lambda_q2, lambda_k2, g_diff_sub, w_out, g_post_mix,
              g_pre_ffn, w_router, b_router, w_gate, w_up, w_down,
              ws_gate, ws_up, ws_down, g_post_ffn):
    offs = []
    acc = 0
    for wdt in IN_SPLITS[:-1]:
        acc += wdt
        offs.append(acc)
    for l in range(DEPTH):
        lambda_init = 0.8 - 0.6 * math.exp(-0.3 * l)
        mod = (jax.nn.silu(c) @ w_ada[l] + b_ada[l])[:, None, :]
        sh_a, sc_a, gt_a, sh_f, sc_f, gt_f = jnp.split(mod, 6, axis=-1)

        h = rmsnorm(x, g_pre_mix[l]) * (1.0 + sc_a) + sh_a
        proj = h @ w_in[l]
        q_lat, kv_lat, k_pe, dq, dk, dv = jnp.split(proj, offs, axis=-1)
        o_mla = mla_mixer(q_lat, kv_lat, k_pe, g_q_lat[l], w_uq[l], g_kv_lat[l], w_ukv[l])
        o_diff = diff_mixer(dq, dk, dv, lambda_q1[l], lambda_k1[l], lambda_q2[l], lambda_k2[l],
                            g_diff_sub[l], lambda_init)
        y = jnp.concatenate([o_mla, o_diff], axis=-1) @ w_out[l]
        x = x + gt_a * rmsnorm(y, g_post_mix[l])

        h = rmsnorm(x, g_pre_ffn[l]) * (1.0 + sc_f) + sh_f
        y = moe(h, w_router[l], b_router[l], w_gate[l], w_up[l], w_down[l],
                ws_gate[l], ws_up[l], ws_down[l])
        x = x + gt_f * rmsnorm(y, g_post_ffn[l])
    return x
```

```python
import contextlib
import math
import numpy as np
import ml_dtypes
import concourse.bass as bass
import concourse.mybir as mybir
from concourse.bass_utils import run_bass_kernel_spmd

F32 = mybir.dt.float32
BF16 = mybir.dt.bfloat16
AF = mybir.ActivationFunctionType
ALU = mybir.AluOpType

ENGS = ("sync", "tensor", "vector", "scalar", "gpsimd")
SEM_WRAP = 30000
NCORES = 8
D = 2048
SEQ = 8192
NOWN = 1024
EPS = 1e-6
THETA = 500000.0
NEXP = 64


class Op:
    __slots__ = ("eng", "fn", "deps", "signal", "dma", "sem", "val")

    def __init__(self, eng, fn, dma):
        self.eng, self.fn, self.dma = eng, fn, dma
        self.deps = []
        self.signal = False
        self.sem = None
        self.val = 0


class Sched:
    def __init__(self):
        self.ops = {e: [] for e in ENGS}
        self.last_w = {}
        self.readers = {}
        self.pending_barrier = {}
        self.dma_since = []

    def op(self, eng, fn, reads=(), writes=(), dma=False):
        o = Op(eng, fn, dma)
        deps = {}
        bank_r = [r for r in reads if isinstance(r, str) and r.startswith("bank")]
        if bank_r:
            reads = [r for r in reads if r not in bank_r]
            writes = list(writes) + bank_r
        for r in reads:
            w = self.last_w.get(r)
            if w is not None:
                deps[id(w)] = w
        for r in writes:
            w = self.last_w.get(r)
            if w is not None:
                deps[id(w)] = w
            for rd in self.readers.get(r, ()):
                deps[id(rd)] = rd
        for r in reads:
            self.readers.setdefault(r, []).append(o)
        for r in writes:
            self.last_w[r] = o
            self.readers[r] = []
        pb = self.pending_barrier.pop(eng, None)
        if pb:
            for d in pb:
                deps[id(d)] = d
        o.deps = [d for d in deps.values() if d is not o]
        for d in o.deps:
            d.signal = True
        self.ops[eng].append(o)
        if dma:
            self.dma_since.append(o)
        return o

    def barrier(self):
        pts = [self.ops[e][-1] for e in ENGS if self.ops[e]] + self.dma_since
        self.dma_since = []
        for e in ENGS:
            self.pending_barrier[e] = list(self.pending_barrier.get(e, [])) + pts
        self.last_w = {}
        self.readers = {}


def run_sched(nc, sched, final_ops, n_dma_sems=16):
    for o in final_ops:
        o.signal = True
    es = contextlib.ExitStack()
    with es:
        eng_sems = {}
        for e in ENGS:
            n_sig = sum(1 for o in sched.ops[e] if o.signal and not o.dma)
            nsem = max(1, (n_sig + SEM_WRAP - 1) // SEM_WRAP)
            eng_sems[e] = [es.enter_context(nc.semaphore(f"s_{e}_{i}")) for i in range(nsem)]
        dma_pool = {e: [es.enter_context(nc.semaphore(f"d_{e}_{i}")) for i in range(n_dma_sems)]
                    for e in ENGS if any(o.dma for o in sched.ops[e])}
        for e in ENGS:
            cnt = 0
            k = 0
            slot_uses = [0] * n_dma_sems
            slot_prev = [None] * n_dma_sems
            di = 0
            for o in sched.ops[e]:
                if o.dma:
                    s = di % n_dma_sems
                    di += 1
                    slot_uses[s] += 1
                    o.sem = dma_pool[e][s]
                    o.val = 16 * slot_uses[s]
                    if slot_prev[s] is not None:
                        o.deps.append(slot_prev[s])
                    slot_prev[s] = o
                    o.signal = True
                elif o.signal:
                    if cnt >= SEM_WRAP:
                        k += 1
                        cnt = 0
                    cnt += 1
                    o.sem = eng_sems[e][k]
                    o.val = cnt
        blk = es.enter_context(nc.Block())

        def make(e):
            def body(eng):
                waited = {}
                for o in sched.ops[e]:
                    for d in o.deps:
                        key = id(d.sem)
                        if waited.get(key, 0) >= d.val:
                            continue
                        eng.wait_ge(d.sem, d.val)
                        waited[key] = d.val
                    ins = o.fn(eng)
                    if o.signal:
                        ins.then_inc(o.sem, 16 if o.dma else 1)
                if e == "sync":
                    for o in final_ops:
                        if waited.get(id(o.sem), 0) < o.val:
                            eng.wait_ge(o.sem, o.val)
                            waited[id(o.sem)] = o.val
            return body

        for e in ENGS:
            getattr(blk, e)(make(e))


class Arena:
    def __init__(self, nc, nbytes):
        self.t = nc.alloc_sbuf_tensor("arena", [128, nbytes // 2], BF16)
        self.cap = nbytes
        self.off = 0

    def alloc(self, shape, dtype):
        esz = 4 if dtype == F32 else 2
        n = int(np.prod(shape[1:]))
        nbytes = (n * esz + 31) // 32 * 32
        assert self.off + nbytes <= self.cap, (self.off, nbytes, self.cap)
        ap = self.t[0:shape[0], self.off // 2:(self.off + n * esz) // 2]
        self.off += nbytes
        if dtype == F32:
            ap = ap.bitcast(F32)
        if len(shape) == 3:
            ap = ap.rearrange("p (a b) -> p a b", a=shape[1])
        elif len(shape) == 4:
            ap = ap.rearrange("p (a b c) -> p a b c", a=shape[1], b=shape[2])
        return ap


def build_program(dbg=False, stop=None, ng1a=None):
    nc = bass.Bass("TRN2", target_bir_lowering=False)
    S = Sched()

    def din(name, shape, dt=F32):
        return nc.dram_tensor(name, list(shape), dt, kind="ExternalInput").ap()

    x_all = din("x_all", [SEQ, D])
    x_own = din("x_own", [NOWN, D])
    c_col = din("c_col", [128, 16])
    w_ada = din("w_ada", [D, 6 * D])
    b_ada_col = din("b_ada_col", [128, 96])
    gcols = din("gcols", [128, 64])
    gq_col = din("gq_col", [128, 6])
    gkv_col = din("gkv_col", [128, 4])
    lam_row = din("lam_row", [1, 256])
    gsub_row = din("gsub_row", [1, 128])
    gsub_col = din("gsub_col", [128, 1])
    brt_row = din("brt_row", [1, 64])
    w_in = din("w_in", [D, 4416])
    w_uq = din("w_uq", [768, 1536])
    w_ukv = din("w_ukv", [512, 2048])
    w_out = din("w_out", [D, D])
    w_router = din("w_router", [D, 64])
    if stop is None:
        w_gate = din("w_gate", [NEXP, D, 512])
        w_up = din("w_up", [NEXP, D, 512])
        w_down = din("w_down", [NEXP, 512, D])
        ws_gate = din("ws_gate", [D, 512])
        ws_up = din("ws_up", [D, 512])
        ws_down = din("ws_down", [512, D])
    ropeK = din("ropeK", [2, 64, SEQ])
    ropeD = din("ropeD", [2, 128, SEQ])
    ropeKq = din("ropeKq", [2, 64, NOWN])
    ropeDq = din("ropeDq", [2, 128, NOWN])
    perms = din("perms", [128, 192], BF16)
    masks = din("masks", [128, 2, 8, 128], BF16)
    out = nc.dram_tensor("out", [NOWN, D], F32, kind="ExternalOutput").ap()

    okind = "ExternalOutput" if dbg else "Internal"
    KT = nc.dram_tensor("KT", [8, 128, SEQ], BF16, kind=okind).ap()
    KPE = nc.dram_tensor("KPE", [64, SEQ], BF16, kind=okind).ap()
    VM = nc.dram_tensor("VM", [SEQ, 8, 128], BF16, kind=okind).ap()
    DKT = nc.dram_tensor("DKT", [8, 128, SEQ], BF16, kind=okind).ap()
    DV = nc.dram_tensor("DV", [SEQ, 8, 128], BF16, kind=okind).ap()
    X1 = nc.dram_tensor("X1", [NOWN, D], F32, kind=okind).ap()
    if dbg:
        OCd = nc.dram_tensor("OCd", [NOWN, D], BF16, kind="ExternalOutput").ap()
        MODd = nc.dram_tensor("MODd", [128, 96], F32, kind="ExternalOutput").ap()
        GATEd = nc.dram_tensor("GATEd", [128, 8, 64], F32, kind="ExternalOutput").ap()

    def sb(name, shape, dt):
        return nc.alloc_sbuf_tensor(name, list(shape), dt)

    ident_f = sb("ident_f", [128, 128], F32)
    ident_b = sb("ident_b", [128, 128], BF16)
    ones_f = sb("ones_f", [128, 128], F32)
    ones_b = sb("ones_b", [128, 128], BF16)
    perm_sb = sb("perm_sb", [128, 192], BF16)
    mask_sb = sb("mask_sb", [128, 2, 8, 128], BF16)
    modc = sb("modc", [128, 96], F32)
    cols = sb("cols", [128, 64], F32)
    gq_sb = sb("gq_sb", [128, 6], F32)
    gkv_sb = sb("gkv_sb", [128, 4], F32)
    gsc_a = sb("gsc_a", [128, 16], F32)
    gsc_f = sb("gsc_f", [128, 16], F32)
    gtg_a = sb("gtg_a", [128, 16], F32)
    gtg_f = sb("gtg_f", [128, 16], F32)
    eps_t = sb("eps_t", [128, 1], F32)
    lamt = sb("lamt", [128, 8], F32)
    lam_in = sb("lam_in", [128, 256], F32)
    lam_junk = sb("lam_junk", [128, 64], F32)
    gsub_sb = sb("gsub_sb", [128, 128], F32)
    brt_sb = sb("brt_sb", [128, 64], F32)
    gsubc = sb("gsubc", [128, 1], F32)
    csil = sb("csil", [128, 16], F32)
    GATE = sb("GATE", [128, 8, 64], F32)
    small = sb("small", [128, 64], F32)

    ARENA_BYTES = 196 * 1024
    A = Arena(nc, ARENA_BYTES)

    banks = [nc.alloc_psum_tensor(f"bank{i}", [128, 512], F32) for i in range(8)]

    def bank_bf(i):
        return banks[i][:].bitcast(BF16)

    sm_ctr = [0]

    def sm():
        i = sm_ctr[0] % 64
        sm_ctr[0] += 1
        return small[:, i:i + 1], ("small", i)

    S.op("gpsimd", lambda e: e.memset(ident_f[:], 0.0), writes=["ident_f"])
    S.op("gpsimd", lambda e: e.affine_select(out=ident_f[:], in_=ident_f[:], compare_op=ALU.not_equal,
                                              fill=1.0, base=0, pattern=[[-1, 128]], channel_multiplier=1),
         reads=["ident_f"], writes=["ident_f"])
    S.op("vector", lambda e: e.tensor_copy(out=ident_b[:], in_=ident_f[:]), reads=["ident_f"], writes=["ident_b"])
    S.op("gpsimd", lambda e: e.memset(ones_f[:], 1.0), writes=["ones_f"])
    S.op("gpsimd", lambda e: e.memset(ones_b[:], 1.0), writes=["ones_b"])
    S.op("gpsimd", lambda e: e.memset(eps_t[:], EPS), writes=["eps"])
    for (dst, src, key) in ((perm_sb[:], perms, "perm"), (mask_sb[:], masks, "mask"), (cols[:], gcols, "cols"),
                            (gq_sb[:], gq_col, "gq"), (gsubc[:], gsub_col, "gsubc"), (gkv_sb[:], gkv_col, "gkv"), (csil[:], c_col, "csil"),
                            (modc[:], b_ada_col, "modb"),
                            (lam_in[:], lam_row.broadcast_to([128, 256]), "lam_in"),
                            (gsub_sb[:], gsub_row.broadcast_to([128, 128]), "gsub"),
                            (brt_sb[:], brt_row.broadcast_to([128, 64]), "brt")):
        S.op("sync", (lambda e, d=dst, s_=src: e.dma_start(out=d, in_=s_)), writes=[key], dma=True)
    S.op("scalar", lambda e: e.activation(out=csil[:], in_=csil[:], func=AF.Silu), reads=["csil"], writes=["csil"])

    lambda_init = 0.8 - 0.6 * math.exp(-0.3 * 0)
    S.op("vector", lambda e: e.tensor_tensor(out=lam_junk[:], in0=lam_in[:, 0:64], in1=lam_in[:, 64:128], op=ALU.mult),
         reads=["lam_in"], writes=["lam_junk"])
    S.op("vector", lambda e: e.reduce_sum(out=lamt[:, 0:1], in_=lam_junk[:], axis=mybir.AxisListType.X),
         reads=["lam_junk"], writes=["lam0"])
    S.op("vector", lambda e: e.tensor_tensor(out=lam_junk[:], in0=lam_in[:, 128:192], in1=lam_in[:, 192:256], op=ALU.mult),
         reads=["lam_in", "lam0"], writes=["lam_junk"])
    S.op("vector", lambda e: e.reduce_sum(out=lamt[:, 1:2], in_=lam_junk[:], axis=mybir.AxisListType.X),
         reads=["lam_junk"], writes=["lam1"])
    S.op("scalar", lambda e: e.activation(out=lamt[:, 2:4], in_=lamt[:, 0:2], func=AF.Exp), reads=["lam0", "lam1"], writes=["lam2"])
    S.op("vector", lambda e: e.tensor_tensor(out=lamt[:, 4:5], in0=lamt[:, 2:3], in1=lamt[:, 3:4], op=ALU.subtract),
         reads=["lam2"], writes=["lam4"])
    S.op("vector", lambda e: e.tensor_scalar(out=lamt[:, 5:6], in0=lamt[:, 4:5], scalar1=lambda_init, scalar2=-1.0,
                                             op0=ALU.add, op1=ALU.mult), reads=["lam4"], writes=["neglam"])
    S.op("vector", lambda e: e.tensor_scalar(out=gsub_sb[:], in0=gsub_sb[:], scalar1=1.0 - lambda_init, scalar2=None,
                                             op0=ALU.mult), reads=["gsub"], writes=["gsub"])
    S.op("vector", lambda e: e.tensor_scalar(out=gsubc[:], in0=gsubc[:], scalar1=1.0 - lambda_init, scalar2=None,
                                             op0=ALU.mult), reads=["gsubc"], writes=["gsubc"])

    wKV = A.alloc([128, 16, 2624], BF16)
    wukv = A.alloc([128, 4, 2048], BF16)
    p1a_mark = A.off
    w_in_v = w_in.rearrange("(kc p) n -> p kc n", p=128)
    for kc in range(16):
        S.op("gpsimd", lambda e, kc=kc: e.dma_start(out=wKV[:, kc, 0:576], in_=w_in_v[:, kc, 768:1344]),
             writes=[("wKV", kc)], dma=True)
        S.op("gpsimd", lambda e, kc=kc: e.dma_start(out=wKV[:, kc, 576:2624], in_=w_in_v[:, kc, 2368:4416]),
             writes=[("wKV", kc, 1)], dma=True)
    S.op("gpsimd", lambda e: e.dma_start(out=wukv, in_=w_ukv.rearrange("(kc p) n -> p kc n", p=128)),
         writes=["wukv"], dma=True)
    acc_ada = A.alloc([128, 6 * D], F32)
    NWB = 6
    wa = [A.alloc([128, 2048], F32) for _ in range(NWB)]
    pmod = banks[0][:, 0:96]
    ci = 0
    for kc in range(16):
        for th in range(6):
            b = ci % NWB
            ci += 1
            S.op("sync" if ci % 2 == 0 else "scalar",
                 (lambda e, kc=kc, b=b, th=th: e.dma_start(out=wa[b], in_=w_ada[kc * 128:(kc + 1) * 128, th * 2048:(th + 1) * 2048])),
                 writes=[("wa", b)], dma=True)
            if kc == 0:
                S.op("vector", lambda e, b=b, th=th: e.tensor_scalar(out=acc_ada[:, th * 2048:(th + 1) * 2048], in0=wa[b], scalar1=csil[:, 0:1],
                                                                      scalar2=None, op0=ALU.mult),
                     reads=[("wa", b), "csil"], writes=[("acc", th)])
            else:
                S.op("vector", lambda e, b=b, th=th, kc=kc: e.scalar_tensor_tensor(
                    out=acc_ada[:, th * 2048:(th + 1) * 2048], in0=wa[b], scalar=csil[:, kc:kc + 1], in1=acc_ada[:, th * 2048:(th + 1) * 2048],
                    op0=ALU.mult, op1=ALU.add), reads=[("wa", b), "csil", ("acc", th)], writes=[("acc", th)])

    def mm_ada(e):
        ins = None
        for j in range(96):
            ins = e.matmul(pmod[:, j:j + 1], lhsT=acc_ada[:, j * 128:(j + 1) * 128], rhs=ones_f[:, 0:1],
                           start=True, stop=True, skip_group_check=True)
        return ins
    S.op("tensor", mm_ada, reads=[("acc", th) for th in range(6)] + ["ones_f"], writes=["bank0"])
    S.op("vector", lambda e: e.tensor_tensor(out=modc[:], in0=pmod, in1=modc[:], op=ALU.add),
         reads=["bank0", "modb"], writes=["modc"])
    dbg_ops = []
    if dbg:
        dbg_ops.append(S.op("sync", lambda e: e.dma_start(out=MODd, in_=modc[:]), reads=["modc"], dma=True))
    S.op("vector", lambda e: e.scalar_tensor_tensor(out=gsc_a[:], in0=modc[:, 16:32], scalar=1.0, in1=cols[:, 0:16],
                                                    op0=ALU.add, op1=ALU.mult), reads=["modc", "cols"], writes=["gsc_a"])
    S.op("vector", lambda e: e.scalar_tensor_tensor(out=gsc_f[:], in0=modc[:, 64:80], scalar=1.0, in1=cols[:, 32:48],
                                                    op0=ALU.add, op1=ALU.mult), reads=["modc", "cols"], writes=["gsc_f"])
    S.op("vector", lambda e: e.tensor_tensor(out=gtg_a[:], in0=modc[:, 32:48], in1=cols[:, 16:32], op=ALU.mult),
         reads=["modc", "cols"], writes=["gtg_a"])
    S.op("vector", lambda e: e.tensor_tensor(out=gtg_f[:], in0=modc[:, 80:96], in1=cols[:, 48:64], op=ALU.mult),
         reads=["modc", "cols"], writes=["gtg_f"])
    S.barrier()
    A.off = 0
    if stop == "p0":
        run_sched(nc, S, dbg_ops)
        return nc

    def rstd_from_ss(ss_ap, ss_key, n, out_ap, out_key):
        S.op("scalar", lambda e: e.activation(out=out_ap, in_=ss_ap, func=AF.Ln, scale=1.0 / n, bias=eps_t[0:ss_ap.shape[0], 0:1]),
             reads=[ss_key, "eps"], writes=[out_key])
        S.op("scalar", lambda e: e.activation(out=out_ap, in_=out_ap, func=AF.Exp, scale=-0.5),
             reads=[out_key], writes=[out_key])

    def load_norm_transpose(src_rows, hT_ap, hT_key, col0, xs, xs_key, xn, xn_key, pbanks, gsc, sh_lo, junk, junk_key):
        S.op("sync", lambda e: e.dma_start(out=xs, in_=src_rows), writes=[xs_key], dma=True)
        ss, ssk = sm()
        rs, rsk = sm()
        S.op("scalar", lambda e: e.activation(out=junk, in_=xs, func=AF.Square, accum_out=ss),
             reads=[xs_key], writes=[junk_key, ssk])
        rstd_from_ss(ss, ssk, D, rs, rsk)
        S.op("vector", lambda e: e.tensor_scalar(out=xn, in0=xs, scalar1=rs, scalar2=None, op0=ALU.mult),
             reads=[xs_key, rsk], writes=[xn_key])
        for hb in range(2):
            pb = pbanks[hb]
            pv = bank_bf(pb)[:, 0:1024].rearrange("p (a b) -> p a b", a=8)

            def tr(e, hb=hb, pv=pv):
                ins = None
                for j in range(8):
                    kc = hb * 8 + j
                    ins = e.transpose(out=pv[:, j, :], in_=xn[:, kc * 128:(kc + 1) * 128], identity=ident_b[:])
                return ins
            S.op("tensor", tr, reads=[xn_key, "ident_b"], writes=[f"bank{pb}"])

            def ev(e, hb=hb, pv=pv):
                ins = None
                for j in range(8):
                    kc = hb * 8 + j
                    ins = e.tensor_scalar(out=hT_ap[:, kc, col0:col0 + 128], in0=pv[:, j, :],
                                          scalar1=gsc[:, kc:kc + 1], scalar2=modc[:, sh_lo + kc:sh_lo + kc + 1],
                                          op0=ALU.mult, op1=ALU.add)
                return ins
            S.op("vector", ev, reads=[f"bank{pb}", "gsc_a", "gsc_f", "modc"], writes=[hT_key])

    def mm_acc(out_ap, pairs, reads, writes):
        def f(e):
            ins = None
            n = len(pairs)
            for i, (l, r) in enumerate(pairs):
                ins = e.matmul(out_ap, lhsT=l, rhs=r, start=(i == 0), stop=(i == n - 1))
            return ins
        return S.op("tensor", f, reads=reads, writes=writes)

    NG = 256
    A.off = p1a_mark
    wKV_keys = [("wKV", kc) for kc in range(16)] + [("wKV", kc, 1) for kc in range(16)]

    xs2 = [A.alloc([128, D], F32) for _ in range(2)]
    junkA = A.alloc([128, D], BF16)
    hT2 = [A.alloc([128, 16, NG], BF16) for _ in range(2)]
    kvraw = A.alloc([128, 4, NG], F32)
    sqb = A.alloc([128, 4, NG], BF16)
    rrep = A.alloc([128, NG], F32)
    kvn = A.alloc([128, 4, NG], BF16)
    kT_out = A.alloc([128, 8, NG], BF16)
    v_out = A.alloc([128, 2, 1024], BF16)
    kpe_bf = A.alloc([64, NG], BF16)
    kpe_out = A.alloc([64, NG], BF16)
    dk_bf = A.alloc([128, 8, NG], BF16)
    dkT_out = A.alloc([128, 8, NG], BF16)
    dv_out = A.alloc([128, 2, 1024], BF16)
    tabK = A.alloc([64, 2, NG], F32)
    tabD = A.alloc([128, 2, NG], F32)
    t1 = A.alloc([128, NG], F32)
    t2 = A.alloc([128, NG], F32)
    permD = perm_sb[:, 0:128]
    permK = perm_sb[0:64, 128:192]

    def rope_apply(src_bf, src_key, ptmp_bank, perm, tab, tab_key, dst, dst_key, np_, t1, t2):
        pt = banks[ptmp_bank][0:np_, 0:NG]
        S.op("tensor", lambda e: e.matmul(pt, lhsT=perm, rhs=src_bf, start=True, stop=True),
             reads=[src_key, "perm"], writes=[f"bank{ptmp_bank}"])
        S.op("vector", lambda e: e.tensor_tensor(out=t2[0:np_, :], in0=pt, in1=tab[:, 1, :], op=ALU.mult),
             reads=[f"bank{ptmp_bank}", tab_key], writes=["t2"])
        S.op("gpsimd", lambda e: e.tensor_tensor(out=t1[0:np_, :], in0=src_bf, in1=tab[:, 0, :], op=ALU.mult),
             reads=[src_key, tab_key], writes=["t1"])
        S.op("vector", lambda e: e.tensor_tensor(out=dst, in0=t1[0:np_, :], in1=t2[0:np_, :], op=ALU.add),
             reads=["t1", "t2"], writes=[dst_key])

    rot = {}

    def next_bank(lst):
        k = tuple(lst)
        i = rot.get(k, 0)
        rot[k] = i + 1
        return lst[i % len(lst)]

    FM_BANKS = [2, 3, 4]
    TM_BANKS = [5, 6]
    kv_finals = []
    n_g1a = ng1a if ng1a else SEQ // NG
    xn4 = [A.alloc([128, D], BF16) for _ in range(4)]

    def prepA_dma(g, src, xs_l, tag):
        for t in range(2):
            xs = xs_l[t][:, :]
            rows = src[g * NG + t * 128: g * NG + (t + 1) * 128, :]
            S.op("sync", lambda e, xs=xs, rows=rows: e.dma_start(out=xs, in_=rows), writes=[(tag + "xs", t)], dma=True)

    def prepA(g, src, xs_l, xn_l, junk, tag):
        for t in range(2):
            xi = (2 * g + t) % len(xn_l)
            xs, xn = xs_l[t][:, :], xn_l[xi][:, :]
            xs_key, xn_key = (tag + "xs", t), (tag + "xn", xi)
            ss, ssk = sm()
            rs, rsk = sm()
            S.op("scalar", lambda e, xs=xs, ss=ss, junk=junk: e.activation(out=junk, in_=xs, func=AF.Square, accum_out=ss),
                 reads=[xs_key], writes=[tag + "junk", ssk])
            rstd_from_ss(ss, ssk, D, rs, rsk)
            S.op("vector", lambda e, xs=xs, xn=xn, rs=rs: e.tensor_scalar(out=xn, in0=xs, scalar1=rs, scalar2=None, op0=ALU.mult),
                 reads=[xs_key, rsk], writes=[xn_key])

    def prepB(g, hT_l, xn_l, tag, tiles=(0, 1)):
        hT = hT_l[g % 2]
        hkey = (tag + "hT", g % 2)
        for t in tiles:
            xi = (2 * g + t) % len(xn_l)
            xn = xn_l[xi][:, :]
            xn_key = (tag + "xn", xi)
            for hb in range(2):
                pb = hb
                pv = bank_bf(pb)[:, 0:1024].rearrange("p (a b) -> p a b", a=8)

                def tr(e, hb=hb, pv=pv, xn=xn):
                    ins = None
                    for j in range(8):
                        kc = hb * 8 + j
                        ins = e.transpose(out=pv[:, j, :], in_=xn[:, kc * 128:(kc + 1) * 128], identity=ident_b[:])
                    return ins
                S.op("tensor", tr, reads=[xn_key, "ident_b"], writes=[f"bank{pb}"])

                def ev(e, hb=hb, pv=pv, hT=hT, t=t):
                    ins = None
                    for j in range(8):
                        kc = hb * 8 + j
                        ins = e.tensor_scalar(out=hT[:, kc, t * 128:(t + 1) * 128], in0=pv[:, j, :],
                                              scalar1=gsc_a[:, kc:kc + 1], scalar2=modc[:, kc:kc + 1],
                                              op0=ALU.mult, op1=ALU.add)
                    return ins
                S.op("vector", ev, reads=[f"bank{pb}", "gsc_a", "modc"], writes=[hkey])

    prepA_dma(0, x_all, xs2, "a")
    prepA(0, x_all, xs2, xn4, junkA[:, :], "a")
    prepB(0, hT2, xn4, "a")
    if n_g1a > 1:
        prepA_dma(1, x_all, xs2, "a")
        prepA(1, x_all, xs2, xn4, junkA[:, :], "a")
    for g in range(n_g1a):
        hb_i = g % 2
        hT = hT2[hb_i]
        hkey = ("ahT", hb_i)
        tok0 = g * NG
        if g + 2 < n_g1a:
            prepA_dma(g + 2, x_all, xs2, "a")
        S.op("sync", lambda e, tok0=tok0: e.dma_start(out=tabK, in_=ropeK[:, :, tok0:tok0 + NG].rearrange("a p t -> p a t")),
             writes=["tabK"], dma=True)
        S.op("sync", lambda e, tok0=tok0: e.dma_start(out=tabD, in_=ropeD[:, :, tok0:tok0 + NG].rearrange("a p t -> p a t")),
             writes=["tabD"], dma=True)
        for c4 in range(4):
            pb = next_bank(FM_BANKS)
            po = banks[pb][:, 0:NG]
            mm_acc(po, [(wKV[:, kc, c4 * 128:(c4 + 1) * 128], hT[:, kc, :]) for kc in range(16)],
                   reads=[hkey] + wKV_keys, writes=[f"bank{pb}"])
            S.op("scalar", lambda e, po=po, c4=c4: e.activation(out=sqb[:, c4, :], in_=po, func=AF.Square),
                 reads=[f"bank{pb}"], writes=[("sqb", c4)])
            S.op("vector", lambda e, po=po, c4=c4: e.tensor_copy(out=kvraw[:, c4, :], in_=po),
                 reads=[f"bank{pb}"], writes=[("kvraw", c4)])
        pb = next_bank(FM_BANKS)
        po = banks[pb][0:64, 0:NG]
        mm_acc(po, [(wKV[:, kc, 512:576], hT[:, kc, :]) for kc in range(16)], reads=[hkey] + wKV_keys, writes=[f"bank{pb}"])
        S.op("scalar", lambda e, po=po: e.copy(out=kpe_bf, in_=po), reads=[f"bank{pb}"], writes=["kpe_bf"])

        def dk_mm(h):
            pb = next_bank(FM_BANKS)
            po = banks[pb][:, 0:NG]
            mm_acc(po, [(wKV[:, kc, 576 + h * 128:576 + (h + 1) * 128], hT[:, kc, :]) for kc in range(16)],
                   reads=[hkey] + wKV_keys, writes=[f"bank{pb}"])
            S.op("scalar", lambda e, po=po, h=h: e.copy(out=dk_bf[:, h, :], in_=po), reads=[f"bank{pb}"], writes=[("dk_bf", h)])

        def dk_rope(h):
            rope_apply(dk_bf[:, h, :], ("dk_bf", h), 7, permD, tabD, "tabD", dkT_out[:, h, :], ("dkT_out", h), 128, t1, t2)
        dk_mm(0)
        pss = banks[7][:, 0:NG]
        mm_acc(pss, [(ones_b[:], sqb[:, c4, :]) for c4 in range(4)],
               reads=[("sqb", c4) for c4 in range(4)] + ["ones_b"], writes=["bank7"])
        S.op("scalar", lambda e, pss=pss: e.activation(out=rrep, in_=pss, func=AF.Ln, scale=1.0 / 512, bias=eps_t[:, 0:1]),
             reads=["bank7", "eps"], writes=["rrep"])
        S.op("scalar", lambda e: e.activation(out=rrep, in_=rrep, func=AF.Exp, scale=-0.5), reads=["rrep"], writes=["rrep"])
        for c4 in range(4):
            S.op("vector", lambda e, c4=c4: e.scalar_tensor_tensor(out=kvn[:, c4, :], in0=kvraw[:, c4, :],
                                                                    scalar=gkv_sb[:, c4:c4 + 1], in1=rrep,
                                                                    op0=ALU.mult, op1=ALU.mult),
                 reads=[("kvraw", c4), "gkv", "rrep"], writes=[("kvn", c4)])
        kvn_keys = [("kvn", c4) for c4 in range(4)]
        rope_apply(kpe_bf, "kpe_bf", 7, permK, tabK, "tabK", kpe_out, "kpe_out", 64, t1, t2)
        kv_finals.append(S.op("sync", lambda e, tok0=tok0: e.dma_start(out=KPE[:, tok0:tok0 + NG], in_=kpe_out), reads=["kpe_out"],
             writes=["KPE"], dma=True))
        for h in range(1, 8):
            dk_mm(h)
            dk_rope(h - 1)
        if g + 2 < n_g1a:
            prepA(g + 2, x_all, xs2, xn4, junkA[:, :], "a")
        if g + 1 < n_g1a:
            prepB(g + 1, hT2, xn4, "a", tiles=(0,))
        first = True
        for t in range(2):
            if t == 1 and g + 1 < n_g1a:
                prepB(g + 1, hT2, xn4, "a", tiles=(1,))
            for cg in range(2):
                pb = next_bank(TM_BANKS)
                po = banks[pb][:, :]
                mm_acc(po, [(hT[:, kc, t * 128:(t + 1) * 128], wKV[:, kc, 1600 + cg * 512:1600 + (cg + 1) * 512]) for kc in range(16)],
                       reads=[hkey] + wKV_keys, writes=[f"bank{pb}"])
                S.op("scalar", lambda e, po=po, t=t, cg=cg: e.copy(out=dv_out[:, t, cg * 512:(cg + 1) * 512], in_=po),
                     reads=[f"bank{pb}"], writes=[("dv_out", t, cg)])
                if first:
                    dk_rope(7)
                    kv_finals.append(S.op("sync", lambda e, tok0=tok0: e.dma_start(out=DKT[:, :, tok0:tok0 + NG].rearrange("h p t -> p h t"), in_=dkT_out),
                         reads=[("dkT_out", h) for h in range(8)], writes=["DKT"], dma=True))
                    first = False
        kv_finals.append(S.op("sync", lambda e, tok0=tok0: e.dma_start(
            out=DV[tok0:tok0 + NG, :, :].rearrange("(t p) h d -> p t (h d)", p=128), in_=dv_out),
            reads=[("dv_out", t, cg) for t in range(2) for cg in range(2)], writes=["DV"], dma=True))
        for h in range(8):
            pb = next_bank(FM_BANKS)
            po = banks[pb][:, 0:NG]
            mm_acc(po, [(wukv[:, c4, h * 256:h * 256 + 128], kvn[:, c4, :]) for c4 in range(4)],
                   reads=kvn_keys + ["wukv"], writes=[f"bank{pb}"])
            S.op("vector", lambda e, po=po, h=h: e.tensor_copy(out=kT_out[:, h, :], in_=po),
                 reads=[f"bank{pb}"], writes=[("kT_out", h)])
        kv_finals.append(S.op("sync", lambda e, tok0=tok0: e.dma_start(out=KT[:, :, tok0:tok0 + NG].rearrange("h p t -> p h t"), in_=kT_out),
             reads=[("kT_out", h) for h in range(8)], writes=["KT"], dma=True))
        wukv_v = wukv.rearrange("p c (h x) -> p c h x", h=8)
        for t in range(2):
            for cg in range(2):
                pb = next_bank(TM_BANKS)
                po = banks[pb][:, :]

                def mmv(e, po=po, t=t, cg=cg):
                    ins = None
                    for hh in range(4):
                        for c4 in range(4):
                            ins = e.matmul(po[:, hh * 128:(hh + 1) * 128], lhsT=kvn[:, c4, t * 128:(t + 1) * 128],
                                           rhs=wukv_v[:, c4, cg * 4 + hh, 128:256], start=(c4 == 0), stop=(c4 == 3))
                    return ins
                S.op("tensor", mmv, reads=kvn_keys + ["wukv"], writes=[f"bank{pb}"])
                S.op("scalar", lambda e, po=po, t=t, cg=cg: e.copy(out=v_out[:, t, cg * 512:(cg + 1) * 512], in_=po),
                     reads=[f"bank{pb}"], writes=[("v_out", t, cg)])
        kv_finals.append(S.op("sync", lambda e, tok0=tok0: e.dma_start(
            out=VM[tok0:tok0 + NG, :, :].rearrange("(t p) h d -> p t (h d)", p=128), in_=v_out),
            reads=[("v_out", t, cg) for t in range(2) for cg in range(2)], writes=["VM"], dma=True))
    S.barrier()
    A.off = 0
    if stop == "p1a":
        run_sched(nc, S, dbg_ops + kv_finals)
        return nc

    KB = 1024
    A.off = 0
    h2T = A.alloc([128, 16, NOWN], BF16)
    OC = A.alloc([128, 8, D], BF16)
    QN = A.alloc([128, 8, NOWN], BF16)
    QPE = A.alloc([64, 8, NOWN], BF16)
    DQ = A.alloc([128, 8, NOWN], BF16)
    assert A.off == 112 * KB
    A.off = 0
    wQ = A.alloc([128, 16, 1792], BF16)
    assert A.off <= 64 * KB
    A.off = 112 * KB
    wuq = A.alloc([128, 6, 1536], BF16)
    for kc in range(16):
        S.op("gpsimd", lambda e, kc=kc: e.dma_start(out=wQ[:, kc, 0:768], in_=w_in_v[:, kc, 0:768]), writes=[("wQ", kc)], dma=True)
        S.op("gpsimd", lambda e, kc=kc: e.dma_start(out=wQ[:, kc, 768:1792], in_=w_in_v[:, kc, 1344:2368]), writes=[("wQ", kc, 1)], dma=True)
    S.op("gpsimd", lambda e: e.dma_start(out=wuq, in_=w_uq.rearrange("(kc p) n -> p kc n", p=128)), writes=["wuq"], dma=True)
    wQ_keys = [("wQ", kc) for kc in range(16)] + [("wQ", kc, 1) for kc in range(16)]
    xs2q = [A.alloc([128, D], F32) for _ in range(2)]
    xn2q = [A.alloc([128, D], BF16) for _ in range(2)]
    junkQb = A.alloc([128, D], BF16)
    hT2q = [A.alloc([128, 16, NG], BF16) for _ in range(2)]
    qraw = A.alloc([128, 6, NG], F32)
    sq6 = A.alloc([128, 6, NG], BF16)
    rrepq_t = A.alloc([128, NG], F32)
    qn = A.alloc([128, 6, NG], BF16)
    qpe_bf = A.alloc([64, NG], BF16)
    dq_bf = A.alloc([128, NG], BF16)
    tabKq_t = A.alloc([64, 2, NG], F32)
    tabDq_t = A.alloc([128, 2, NG], F32)
    t1q = A.alloc([128, NG], F32)
    t2q = A.alloc([128, NG], F32)
    qpe_bf2 = [qpe_bf, A.alloc([64, NG], BF16)]
    dq_bf2 = [dq_bf, A.alloc([128, NG], BF16)]
    _save = A.off
    A.off = 56 * KB
    xn4q = xn2q + [A.alloc([128, D], BF16) for _ in range(2)]
    assert A.off <= 64 * KB
    A.off = _save
    n_gq = NOWN // NG
    prepA_dma(0, x_own, xs2q, "q")
    prepA(0, x_own, xs2q, xn4q, junkQb[:, :], "q")
    prepB(0, hT2q, xn4q, "q")
    prepA_dma(1, x_own, xs2q, "q")
    prepA(1, x_own, xs2q, xn4q, junkQb[:, :], "q")
    for g in range(n_gq):
        hb_i = g % 2
        hT = hT2q[hb_i]
        hkey = ("qhT", hb_i)
        tok0 = g * NG
        if g + 2 < n_gq:
            prepA_dma(g + 2, x_own, xs2q, "q")
        S.op("sync", lambda e, tok0=tok0: e.dma_start(out=tabKq_t, in_=ropeKq[:, :, tok0:tok0 + NG].rearrange("a p t -> p a t")),
             writes=["tabKq"], dma=True)
        S.op("sync", lambda e, tok0=tok0: e.dma_start(out=tabDq_t, in_=ropeDq[:, :, tok0:tok0 + NG].rearrange("a p t -> p a t")),
             writes=["tabDq"], dma=True)
        for c6 in range(6):
            pb = next_bank(FM_BANKS)
            po = banks[pb][:, 0:NG]
            mm_acc(po, [(wQ[:, kc, c6 * 128:(c6 + 1) * 128], hT[:, kc, :]) for kc in range(16)],
                   reads=[hkey] + wQ_keys, writes=[f"bank{pb}"])
            S.op("scalar", lambda e, po=po, c6=c6: e.activation(out=sq6[:, c6, :], in_=po, func=AF.Square),
                 reads=[f"bank{pb}"], writes=[("sq6", c6)])
            S.op("vector", lambda e, po=po, c6=c6: e.tensor_copy(out=qraw[:, c6, :], in_=po),
                 reads=[f"bank{pb}"], writes=[("qraw", c6)])

        def dq_mm(h):
            pb = next_bank(FM_BANKS)
            po = banks[pb][:, 0:NG]
            mm_acc(po, [(wQ[:, kc, 768 + h * 128:768 + (h + 1) * 128], hT[:, kc, :]) for kc in range(16)],
                   reads=[hkey] + wQ_keys, writes=[f"bank{pb}"])
            S.op("scalar", lambda e, po=po, h=h: e.copy(out=dq_bf2[h % 2], in_=po), reads=[f"bank{pb}"], writes=[("dq_bf", h % 2)])

        def dq_rope(h):
            rope_apply(dq_bf2[h % 2], ("dq_bf", h % 2), 7, permD, tabDq_t, "tabDq", DQ[:, h, tok0:tok0 + NG], ("DQ", h, g), 128, t1q, t2q)
        dq_mm(0)
        pss = banks[6][:, 0:NG]
        mm_acc(pss, [(ones_b[:], sq6[:, c6, :]) for c6 in range(6)],
               reads=[("sq6", c6) for c6 in range(6)] + ["ones_b"], writes=["bank6"])
        S.op("scalar", lambda e, pss=pss: e.activation(out=rrepq_t, in_=pss, func=AF.Ln, scale=1.0 / 768, bias=eps_t[:, 0:1]),
             reads=["bank6", "eps"], writes=["rrepq"])
        S.op("scalar", lambda e: e.activation(out=rrepq_t, in_=rrepq_t, func=AF.Exp, scale=-0.5), reads=["rrepq"], writes=["rrepq"])
        for c6 in range(6):
            S.op("vector", lambda e, c6=c6: e.scalar_tensor_tensor(out=qn[:, c6, :], in0=qraw[:, c6, :],
                                                                    scalar=gq_sb[:, c6:c6 + 1], in1=rrepq_t,
                                                                    op0=ALU.mult, op1=ALU.mult),
                 reads=[("qraw", c6), "gq", "rrepq"], writes=[("qn", c6)])
        qn_keys = [("qn", c6) for c6 in range(6)]
        for h in range(1, 8):
            dq_mm(h)
            dq_rope(h - 1)
        if g + 2 < n_gq:
            prepA(g + 2, x_own, xs2q, xn4q, junkQb[:, :], "q")
        if g + 1 < n_gq:
            prepB(g + 1, hT2q, xn4q, "q")

        def qpe_rope(h):
            rope_apply(qpe_bf2[h % 2], ("qpe_bf", h % 2), 7, permK, tabKq_t, "tabKq", QPE[:, h, tok0:tok0 + NG], ("QPE", h, g), 64, t1q, t2q)
        for h in range(8):
            pb = next_bank(FM_BANKS)
            po = banks[pb][:, 0:NG]
            mm_acc(po, [(wuq[:, c6, h * 192:h * 192 + 128], qn[:, c6, :]) for c6 in range(6)],
                   reads=qn_keys + ["wuq"], writes=[f"bank{pb}"])
            S.op("vector", lambda e, po=po, h=h, tok0=tok0: e.tensor_copy(out=QN[:, h, tok0:tok0 + NG], in_=po),
                 reads=[f"bank{pb}"], writes=[("QN", h, g)])
            if h == 0:
                dq_rope(7)
            pb = next_bank(FM_BANKS)
            po = banks[pb][0:64, 0:NG]
            mm_acc(po, [(wuq[:, c6, h * 192 + 128:h * 192 + 192], qn[:, c6, :]) for c6 in range(6)],
                   reads=qn_keys + ["wuq"], writes=[f"bank{pb}"])
            S.op("scalar", lambda e, po=po, h=h: e.copy(out=qpe_bf2[h % 2], in_=po), reads=[f"bank{pb}"], writes=[("qpe_bf", h % 2)])
            if h > 0:
                qpe_rope(h - 1)
        qpe_rope(7)
    S.barrier()
    A.off = 112 * KB

    OCT = OC.rearrange("p s d -> p (s d)").rearrange("p (k t) -> p k t", k=16)
    A.off = 0
    Vb = [A.alloc([128, 64, 128], BF16) for _ in range(2)]
    assert A.off <= 32 * KB
    A.off = 112 * KB
    KTb = [A.alloc([128, SEQ], BF16) for _ in range(2)]
    KPEs = A.alloc([64, SEQ], BF16)
    NPT = 8
    PT = [A.alloc([128, 512], BF16) for _ in range(NPT)]
    Psum2 = [A.alloc([128, NOWN], F32) for _ in range(2)]
    rl = A.alloc([128, NOWN], F32)
    osb0 = A.alloc([128, NOWN], F32)
    otmp = A.alloc([128, NOWN], F32)
    sqd = A.alloc([128, NOWN], BF16)
    rstd_t = A.alloc([128, NOWN], F32)
    S.op("sync", lambda e: e.dma_start(out=KPEs, in_=KPE), writes=["KPEs"], dma=True)

    S_BANKS = [0, 1, 2, 5, 6]
    sc_mla = (128 + 64) ** -0.5
    sc_diff = 64 ** -0.5
    pt_i = [0]

    def load_head(hh):
        is_mla = hh < 8
        h = hh % 8
        kb_i = hh % 2
        Ksrc = KT if is_mla else DKT
        Vsrc = VM if is_mla else DV
        Kt = KTb[kb_i]
        Vt = Vb[kb_i]
        for q4 in range(4):
            S.op("sync", lambda e, Kt=Kt, Ksrc=Ksrc, h=h, q4=q4: e.dma_start(
                out=Kt[:, q4 * 2048:(q4 + 1) * 2048], in_=Ksrc[h, :, q4 * 2048:(q4 + 1) * 2048]),
                writes=[("Kt", kb_i, q4)], dma=True)
            S.op("sync", lambda e, Vt=Vt, Vsrc=Vsrc, h=h, q4=q4: e.dma_start(
                out=Vt[:, q4 * 16:(q4 + 1) * 16, :],
                in_=Vsrc[q4 * 2048:(q4 + 1) * 2048, h, :].rearrange("(b p) d -> p b d", p=128)),
                writes=[("Vt", kb_i, q4)], dma=True)

    def denominators(Psum, pi_):
        for half in range(2):
            pb = 7
            S.op("tensor", lambda e, pb=pb, half=half, Psum=Psum: e.matmul(banks[pb][:, :], lhsT=ones_f[:], rhs=Psum[:, half * 512:(half + 1) * 512],
                                                                 start=True, stop=True),
                 reads=[("Psum", pi_, half), "ones_f"], writes=[f"bank{pb}"])
            S.op("vector", lambda e, pb=pb, half=half: e.reciprocal(out=rl[:, half * 512:(half + 1) * 512], in_=banks[pb][:, :]),
                 reads=[f"bank{pb}"], writes=[("rl", half)])

    def evac(hh, comp, Psum, pi_):
        is_mla = hh < 8
        h = hh % 8
        denominators(Psum, pi_)
        if is_mla:
            for half in range(2):
                S.op("vector", lambda e, half=half, h=h: e.tensor_tensor(
                    out=OCT[:, h, half * 512:(half + 1) * 512], in0=banks[3 + half][:, :], in1=rl[:, half * 512:(half + 1) * 512], op=ALU.mult),
                    reads=[f"bank{3 + half}", ("rl", half)], writes=[("OCT", hh, half)])
        elif comp == 0:
            for half in range(2):
                S.op("vector", lambda e, half=half: e.tensor_tensor(
                    out=osb0[:, half * 512:(half + 1) * 512], in0=banks[3 + half][:, :], in1=rl[:, half * 512:(half + 1) * 512], op=ALU.mult),
                    reads=[f"bank{3 + half}", ("rl", half)], writes=[("osb0", half)])
        else:
            for half in range(2):
                hs = slice(half * 512, (half + 1) * 512)
                S.op("vector", lambda e, half=half, hs=hs: e.tensor_tensor(
                    out=otmp[:, hs], in0=banks[3 + half][:, :], in1=rl[:, hs], op=ALU.mult),
                    reads=[f"bank{3 + half}", ("rl", half)], writes=[("otmp", half)])
                S.op("vector", lambda e, hs=hs: e.scalar_tensor_tensor(out=otmp[:, hs], in0=otmp[:, hs], scalar=lamt[:, 5:6], in1=osb0[:, hs],
                                                                        op0=ALU.mult, op1=ALU.add),
                     reads=[("otmp", half), ("osb0", half), "neglam"], writes=[("otmp", half)])
                S.op("scalar", lambda e, hs=hs: e.activation(out=sqd[:, hs], in_=otmp[:, hs], func=AF.Square),
                     reads=[("otmp", half)], writes=[("sqd", half)])
                pb = 7
                S.op("tensor", lambda e, pb=pb, hs=hs: e.matmul(banks[pb][:, :], lhsT=ones_b[:], rhs=sqd[:, hs], start=True, stop=True),
                     reads=[("sqd", half), "ones_b"], writes=[f"bank{pb}"])
                S.op("scalar", lambda e, pb=pb, hs=hs: e.activation(out=rstd_t[:, hs], in_=banks[pb][:, :], func=AF.Ln, scale=1.0 / 128,
                                                                     bias=eps_t[:, 0:1]),
                     reads=[f"bank{pb}", "eps"], writes=[("rstd_t", half)])
                S.op("scalar", lambda e, hs=hs: e.activation(out=rstd_t[:, hs], in_=rstd_t[:, hs], func=AF.Exp, scale=-0.5),
                     reads=[("rstd_t", half)], writes=[("rstd_t", half)])
                S.op("vector", lambda e, hs=hs, h=h: e.scalar_tensor_tensor(
                    out=OCT[:, 8 + h, hs], in0=otmp[:, hs], scalar=gsubc[:, 0:1], in1=rstd_t[:, hs], op0=ALU.mult, op1=ALU.mult),
                    reads=[("otmp", half), ("rstd_t", half), "gsubc"], writes=[("OCT", hh, half)])

    LOOK = 2
    GRP = 2
    pend = []
    dev = [None]
    hc_i = 0
    load_head(0)
    for hh in range(16):
        is_mla = hh < 8
        h = hh % 8
        kb_i = hh % 2
        Kt = KTb[kb_i]
        Vt = Vb[kb_i]
        ncomp = 1 if is_mla else 2
        for comp in range(ncomp):
            pi_ = hc_i % 2
            Psum = Psum2[pi_]
            hc_i += 1
            first_o = {3: True, 4: True}
            clist = []
            for kb in range(64):
                q0 = (kb // 8) * 128
                if q0 < 512:
                    clist.append((kb, q0, 512, q0))
                clist.append((kb, max(q0, 512), 1024, q0))
            nsc = 0
            for g0 in range(0, len(clist), GRP):
                grp = []
                for (kb, qa, qe, q0) in clist[g0:g0 + GRP]:
                    n = qe - qa
                    sb_ = next_bank(S_BANKS)
                    pi = pt_i[0] % NPT
                    pt_i[0] += 1
                    obank = 3 + qa // 512
                    st = first_o[obank]
                    first_o[obank] = False
                    grp.append(dict(kb=kb, qa=qa, qe=qe, q0=q0, n=n, sb=sb_, sbank=banks[sb_][:, 0:n], pi=pi, pt=PT[pi][:, 0:n],
                                    ptk=("PT", pi), obank=obank, st=st, q4=kb // 16, half=qa // 512))

                def smm(e, grp=grp, Kt=Kt, h=h, comp=comp, is_mla=is_mla):
                    ins = None
                    for c in grp:
                        kb, qa, qe, sbank = c["kb"], c["qa"], c["qe"], c["sbank"]
                        if is_mla:
                            e.matmul(sbank, lhsT=Kt[:, kb * 128:(kb + 1) * 128], rhs=QN[:, h, qa:qe], start=True, stop=False)
                            ins = e.matmul(sbank, lhsT=KPEs[:, kb * 128:(kb + 1) * 128], rhs=QPE[:, h, qa:qe], start=False, stop=True)
                        else:
                            lo = comp * 64
                            ins = e.matmul(sbank, lhsT=Kt[lo:lo + 64, kb * 128:(kb + 1) * 128], rhs=DQ[lo:lo + 64, h, qa:qe],
                                           start=True, stop=True)
                    return ins
                S.op("tensor", smm, reads=[("Kt", kb_i, c["q4"]) for c in grp] + ["KPEs"], writes=[f"bank{c['sb']}" for c in grp])
                for c in grp:
                    S.op("scalar", lambda e, c=c, is_mla=is_mla: e.activation(
                        out=c["pt"], in_=c["sbank"], func=AF.Exp, scale=(sc_mla if is_mla else sc_diff)),
                        reads=[f"bank{c['sb']}"], writes=[c["ptk"]])
                    if c["qa"] == c["q0"]:
                        S.op("gpsimd", lambda e, c=c: e.tensor_tensor(
                            out=c["pt"][:, 0:128], in0=c["pt"][:, 0:128], in1=mask_sb[:, (c["kb"] // 8) % 2, c["kb"] % 8, :], op=ALU.mult),
                            reads=[c["ptk"], "mask"], writes=[c["ptk"]])
                    if c["kb"] == 0:
                        S.op("vector", lambda e, c=c, Psum=Psum: e.tensor_copy(out=Psum[:, c["qa"]:c["qe"]], in_=c["pt"]),
                             reads=[c["ptk"]], writes=[("Psum", pi_, c["half"])])
                    else:
                        S.op("vector", lambda e, c=c, Psum=Psum: e.tensor_tensor(out=Psum[:, c["qa"]:c["qe"]], in0=Psum[:, c["qa"]:c["qe"]],
                                                                                 in1=c["pt"], op=ALU.add),
                             reads=[c["ptk"], ("Psum", pi_, c["half"])], writes=[("Psum", pi_, c["half"])])
                nsc += 1
                if nsc == LOOK:
                    if dev[0] is not None:
                        while pend and pend[0][0] != hc_i:
                            pend.pop(0)[1]()
                        dev[0]()
                        dev[0] = None
                    if hh + 1 < 16 and comp == 0:
                        load_head(hh + 1)

                def emit_pv(grp=grp, Vt=Vt, kb_i=kb_i):
                    def pvmm(e):
                        ins = None
                        for c in grp:
                            c0 = c["qa"] % 512
                            ins = e.matmul(banks[c["obank"]][:, c0:c0 + c["n"]], lhsT=Vt[:, c["kb"], :], rhs=c["pt"],
                                           start=c["st"], stop=(c["kb"] == 63), skip_group_check=True)
                        return ins
                    S.op("tensor", pvmm, reads=[c["ptk"] for c in grp] + [("Vt", kb_i, c["q4"]) for c in grp],
                         writes=sorted({f"bank{c['obank']}" for c in grp}))
                pend.append((hc_i, emit_pv))
                if dev[0] is None:
                    while len(pend) > LOOK:
                        pend.pop(0)[1]()
            dev[0] = (lambda hh=hh, comp=comp, Psum=Psum, pi_=pi_: evac(hh, comp, Psum, pi_))
    while pend:
        pend.pop(0)[1]()
    dev[0]()
    if dbg:
        dbg_ops.append(S.op("sync", lambda e: e.dma_start(out=OCd.rearrange("(s p) d -> p s d", p=128), in_=OC), dma=True))
    S.barrier()
    if stop == "p2":
        run_sched(nc, S, dbg_ops)
        return nc
    A.off = 64 * KB

    wo = A.alloc([128, 16, D], BF16)
    wr = A.alloc([128, 16, 64], F32)
    gtgA_rep = A.alloc([128, D], F32)
    diagt = A.alloc([128, 128], F32)
    ocT = A.alloc([128, 16, 128], BF16)
    xo = A.alloc([128, D], F32)
    x1t = A.alloc([128, D], F32)
    xn_f = A.alloc([128, D], F32)
    yjunk = A.alloc([128, D], BF16)
    h2f = A.alloc([128, 16, 128], F32)
    sc_t = A.alloc([128, 64], F32)
    bi_t = A.alloc([128, 64], F32)
    mk_t = A.alloc([128, 64], F32)
    m8 = A.alloc([128, 8, 8], F32)
    gs_t = A.alloc([128, 8], F32)
    gm_t = A.alloc([128, 8], F32)
    pen_t = A.alloc([128, 8], F32)
    wjunk = A.alloc([128, 64], F32)
    S.op("gpsimd", lambda e: e.dma_start(out=wo, in_=w_out.rearrange("(kc p) n -> p kc n", p=128)), writes=["wo"], dma=True)
    S.op("sync", lambda e: e.dma_start(out=wr, in_=w_router.rearrange("(kc p) n -> p kc n", p=128)), writes=["wr"], dma=True)

    def build_rep(gcol, gkey, rep, repkey, diagt):
        for c in range(16):
            S.op("vector", lambda e, c=c: e.tensor_scalar(out=diagt, in0=ident_f[:], scalar1=gcol[:, c:c + 1], scalar2=None, op0=ALU.mult),
                 reads=["ident_f", gkey], writes=["diagt"])
            pb = 7
            po = banks[pb][:, 0:128]
            S.op("tensor", lambda e, po=po: e.matmul(po, lhsT=ones_f[:], rhs=diagt, start=True, stop=True),
                 reads=["diagt", "ones_f"], writes=[f"bank{pb}"])
            S.op("vector", lambda e, po=po, c=c: e.tensor_copy(out=rep[:, c * 128:(c + 1) * 128], in_=po),
                 reads=[f"bank{pb}"], writes=[repkey])
    build_rep(gtg_a, "gtg_a", gtgA_rep, "gtgA_rep", diagt)

    for s in range(8):
        S.op("sync", lambda e, s=s: e.dma_start(out=xo, in_=x_own[s * 128:(s + 1) * 128, :]), writes=["xo"], dma=True)
        ssp = [sm() for _ in range(4)]
        for cg in range(4):
            pb = 2 + cg
            po = banks[pb][:, :]
            mm_acc(po, [(OCT[:, kc, s * 128:(s + 1) * 128], wo[:, kc, cg * 512:(cg + 1) * 512]) for kc in range(16)],
                   reads=["wo"], writes=[f"bank{pb}"])
            S.op("scalar", lambda e, po=po, cg=cg, acc=ssp[cg][0]: e.activation(out=yjunk[:, cg * 512:(cg + 1) * 512], in_=po, func=AF.Square,
                                                                 accum_out=acc),
                 reads=[f"bank{pb}"], writes=[("yjunk", cg), ssp[cg][1]])
        ss, ssk = sm()
        rs, rsk = sm()
        S.op("vector", lambda e, ss=ss, a0=ssp[0][0], a1=ssp[1][0]: e.tensor_tensor(out=ss, in0=a0, in1=a1, op=ALU.add),
             reads=[ssp[0][1], ssp[1][1]], writes=[ssk])
        S.op("vector", lambda e, ss=ss, a2=ssp[2][0]: e.tensor_tensor(out=ss, in0=ss, in1=a2, op=ALU.add), reads=[ssk, ssp[2][1]], writes=[ssk])
        S.op("vector", lambda e, ss=ss, a3=ssp[3][0]: e.tensor_tensor(out=ss, in0=ss, in1=a3, op=ALU.add), reads=[ssk, ssp[3][1]], writes=[ssk])
        rstd_from_ss(ss, ssk, D, rs, rsk)
        for cg in range(4):
            pb = 2 + cg
            po = banks[pb][:, :]
            S.op("vector", lambda e, po=po, cg=cg, rs=rs: e.scalar_tensor_tensor(
                out=x1t[:, cg * 512:(cg + 1) * 512], in0=po, scalar=rs, in1=gtgA_rep[:, cg * 512:(cg + 1) * 512],
                op0=ALU.mult, op1=ALU.mult), reads=[f"bank{pb}", rsk, "gtgA_rep"], writes=[("x1t", cg)])
        S.op("gpsimd", lambda e: e.tensor_tensor(out=x1t, in0=x1t, in1=xo, op=ALU.add),
             reads=[("x1t", cg) for cg in range(4)] + ["xo"], writes=["x1t"])
        S.op("sync", lambda e, s=s: e.dma_start(out=X1[s * 128:(s + 1) * 128, :], in_=x1t), reads=["x1t"], writes=["X1"], dma=True)
        ss2, ss2k = sm()
        rs2, rs2k = sm()
        S.op("scalar", lambda e, ss2=ss2: e.activation(out=yjunk, in_=x1t, func=AF.Square, accum_out=ss2),
             reads=["x1t"], writes=[("yjunk", 0), ("yjunk", 1), ("yjunk", 2), ("yjunk", 3), ss2k])
        rstd_from_ss(ss2, ss2k, D, rs2, rs2k)
        S.op("vector", lambda e, rs2=rs2: e.tensor_scalar(out=xn_f, in0=x1t, scalar1=rs2, scalar2=None, op0=ALU.mult),
             reads=["x1t", rs2k], writes=["xn_f"])
        for q in range(4):
            pb = 2 + q
            pv = banks[pb][:, :].rearrange("p (a b) -> p a b", a=4)

            def trf(e, q=q, pv=pv):
                ins = None
                for j in range(4):
                    kc = q * 4 + j
                    ins = e.transpose(out=pv[:, j, :], in_=xn_f[:, kc * 128:(kc + 1) * 128], identity=ident_f[:])
                return ins
            S.op("tensor", trf, reads=["xn_f", "ident_f"], writes=[f"bank{pb}"])

            def evf(e, q=q, pv=pv):
                ins = None
                for j in range(4):
                    kc = q * 4 + j
                    ins = e.tensor_scalar(out=h2f[:, kc, :], in0=pv[:, j, :], scalar1=gsc_f[:, kc:kc + 1],
                                          scalar2=modc[:, 48 + kc:48 + kc + 1], op0=ALU.mult, op1=ALU.add)
                return ins
            S.op("vector", evf, reads=[f"bank{pb}", "gsc_f", "modc"], writes=[("h2f", q)])
            S.op("gpsimd", lambda e, q=q, s=s: e.tensor_copy(out=h2T[:, q * 4:(q + 1) * 4, s * 128:(s + 1) * 128], in_=h2f[:, q * 4:(q + 1) * 4, :]),
                 reads=[("h2f", q)], writes=[("h2T", s, q)])
        pl = banks[6][:, 0:64]
        mm_acc(pl, [(h2f[:, kc, :], wr[:, kc, :]) for kc in range(16)], reads=[("h2f", q) for q in range(4)] + ["wr"], writes=["bank6"])
        S.op("scalar", lambda e: e.activation(out=sc_t, in_=pl, func=AF.Sigmoid), reads=["bank6"], writes=["sc_t"])
        S.op("vector", lambda e: e.tensor_tensor(out=bi_t, in0=sc_t, in1=brt_sb[:], op=ALU.add), reads=["sc_t", "brt"], writes=["bi_t"])

        def gtop(e):
            ins = None
            for gi in range(8):
                ins = e.max(out=m8[:, gi, :], in_=bi_t[:, gi * 8:(gi + 1) * 8])
            return ins
        S.op("vector", gtop, reads=["bi_t"], writes=["m8"])
        S.op("vector", lambda e: e.tensor_tensor(out=gs_t, in0=m8[:, :, 0], in1=m8[:, :, 1], op=ALU.add), reads=["m8"], writes=["gs_t"])
        S.op("vector", lambda e: e.max(out=m8[:, 0, :], in_=gs_t), reads=["gs_t", "m8"], writes=["m8b"])
        S.op("vector", lambda e: e.tensor_scalar(out=gm_t, in0=gs_t, scalar1=m8[:, 0, 3:4], scalar2=None, op0=ALU.is_ge),
             reads=["gs_t", "m8b"], writes=["gm_t"])
        S.op("vector", lambda e: e.tensor_scalar(out=pen_t, in0=gm_t, scalar1=-1.0, scalar2=1e30, op0=ALU.add, op1=ALU.mult),
             reads=["gm_t"], writes=["pen_t"])

        def mk(e):
            ins = None
            for gi in range(8):
                ins = e.tensor_scalar(out=mk_t[:, gi * 8:(gi + 1) * 8], in0=bi_t[:, gi * 8:(gi + 1) * 8],
                                      scalar1=gm_t[:, gi:gi + 1], scalar2=pen_t[:, gi:gi + 1], op0=ALU.mult, op1=ALU.add)
            return ins
        S.op("vector", mk, reads=["bi_t", "gm_t", "pen_t"], writes=["mk_t"])
        S.op("vector", lambda e: e.max(out=m8[:, 1, :], in_=mk_t), reads=["mk_t", "m8b"], writes=["m8c"])
        S.op("vector", lambda e: e.tensor_scalar(out=mk_t, in0=mk_t, scalar1=m8[:, 1, 7:8], scalar2=None, op0=ALU.is_ge),
             reads=["mk_t", "m8c"], writes=["mk_t"])
        ws_, wsk = sm()
        S.op("vector", lambda e, s=s: e.tensor_tensor(out=GATE[:, s, :], in0=mk_t, in1=sc_t, op=ALU.mult),
             reads=["mk_t", "sc_t"], writes=[("GATE", s)])
        S.op("vector", lambda e, s=s, ws_=ws_: e.reduce_sum(out=ws_, in_=GATE[:, s, :], axis=mybir.AxisListType.X),
             reads=[("GATE", s)], writes=[wsk])
        S.op("vector", lambda e, ws_=ws_: e.reciprocal(out=ws_, in_=ws_), reads=[wsk], writes=[wsk])
        S.op("vector", lambda e, s=s, ws_=ws_: e.tensor_scalar(out=GATE[:, s, :], in0=GATE[:, s, :], scalar1=ws_, scalar2=2.5,
                                                              op0=ALU.mult, op1=ALU.mult), reads=[("GATE", s), wsk], writes=[("GATE", s)])
    if dbg:
        dbg_ops.append(S.op("sync", lambda e: e.dma_start(out=GATEd, in_=GATE[:]), reads=[("GATE", s) for s in range(8)], dma=True))
    if stop == "p3":
        S.barrier()
        run_sched(nc, S, dbg_ops)
        return nc
    h2T_keys = [("h2T", s, q) for s in range(8) for q in range(4)]
    S.barrier()
    A.off = 32 * KB
    GT = A.alloc([64, NOWN], F32)
    Yacc = A.alloc([128, 8, D], F32)
    AT = A.alloc([128, 4, NOWN], BF16)
    Wg = [A.alloc([128, 16, 512], BF16) for _ in range(2)]
    Wu = [A.alloc([128, 16, 512], BF16) for _ in range(2)]
    Wd = [A.alloc([128, 4, D], BF16) for _ in range(1)]
    sg_t = [A.alloc([128, 512], F32) for _ in range(2)]
    grep = A.alloc([128, NOWN], F32)
    for s in range(8):
        pb = 7
        po = banks[pb][0:64, 0:128]
        S.op("tensor", lambda e, po=po, s=s: e.transpose(out=po, in_=GATE[:, s, :], identity=ident_f[:]),
             reads=["ident_f"], writes=[f"bank{pb}"])
        S.op("vector", lambda e, po=po, s=s: e.tensor_copy(out=GT[:, s * 128:(s + 1) * 128], in_=po), reads=[f"bank{pb}"], writes=["GT"])

    for ei in range(NEXP + 1):
        b = ei % 2
        if ei < NEXP:
            gsrc, usrc, dsrc = w_gate[ei], w_up[ei], w_down[ei]
        else:
            gsrc, usrc, dsrc = ws_gate, ws_up, ws_down
        for q in range(2):
            S.op("gpsimd", lambda e, b=b, gsrc=gsrc, q=q: e.dma_start(
                out=Wg[b][:, q * 8:(q + 1) * 8, :], in_=gsrc.rearrange("(kc p) n -> p kc n", p=128)[:, q * 8:(q + 1) * 8, :]),
                writes=[("Wg", b, q)], dma=True)
            S.op("gpsimd", lambda e, b=b, usrc=usrc, q=q: e.dma_start(
                out=Wu[b][:, q * 8:(q + 1) * 8, :], in_=usrc.rearrange("(kc p) n -> p kc n", p=128)[:, q * 8:(q + 1) * 8, :]),
                writes=[("Wu", b, q)], dma=True)
        S.op("gpsimd", lambda e, dsrc=dsrc: e.dma_start(out=Wd[0], in_=dsrc.rearrange("(kc p) n -> p kc n", p=128)),
             writes=["Wd"], dma=True)
        if ei < NEXP:
            for half in range(2):
                pb = 7
                po = banks[pb][:, :]
                S.op("tensor", lambda e, po=po, ei=ei, half=half: e.matmul(
                    po, lhsT=ident_f[0:64, ei:ei + 1].broadcast_to([64, 128]), rhs=GT[:, half * 512:(half + 1) * 512],
                    start=True, stop=True), reads=["GT", "ident_f"], writes=[f"bank{pb}"])
                S.op("vector", lambda e, po=po, half=half: e.tensor_copy(out=grep[:, half * 512:(half + 1) * 512], in_=po),
                     reads=[f"bank{pb}"], writes=[("grep", half)])
        for half in range(2):
            for hc in range(4):
                pg = next_bank([0, 1])
                pu = next_bank([2, 3])
                mm_acc(banks[pg][:, :], [(Wg[b][:, kc, hc * 128:(hc + 1) * 128], h2T[:, kc, half * 512:(half + 1) * 512]) for kc in range(16)],
                       reads=[("Wg", b, 0), ("Wg", b, 1)], writes=[f"bank{pg}"])
                mm_acc(banks[pu][:, :], [(Wu[b][:, kc, hc * 128:(hc + 1) * 128], h2T[:, kc, half * 512:(half + 1) * 512]) for kc in range(16)],
                       reads=[("Wu", b, 0), ("Wu", b, 1)], writes=[f"bank{pu}"])
                sgi = (half * 4 + hc) % 2
                sg = sg_t[sgi]
                S.op("scalar", lambda e, pg=pg, sg=sg: e.activation(out=sg, in_=banks[pg][:, :], func=AF.Silu),
                     reads=[f"bank{pg}"], writes=[("sg", sgi)])
                if ei < NEXP:
                    S.op("gpsimd", lambda e, sg=sg, half=half: e.tensor_tensor(out=sg, in0=sg, in1=grep[:, half * 512:(half + 1) * 512], op=ALU.mult),
                         reads=[("sg", sgi), ("grep", half)], writes=[("sg", sgi)])
                S.op("vector", lambda e, pu=pu, sg=sg, hc=hc, half=half: e.tensor_tensor(
                    out=AT[:, hc, half * 512:(half + 1) * 512], in0=banks[pu][:, :], in1=sg, op=ALU.mult),
                    reads=[f"bank{pu}", ("sg", sgi)], writes=[("AT", hc, half)])
        for tile in range(8):
            for cg in range(4):
                pb = next_bank([4, 5, 6])
                po = banks[pb][:, :]
                mm_acc(po, [(AT[:, hc, tile * 128:(tile + 1) * 128], Wd[0][:, hc, cg * 512:(cg + 1) * 512]) for hc in range(4)],
                       reads=[("AT", hc, tile // 4) for hc in range(4)] + ["Wd"], writes=[f"bank{pb}"])
                if ei == 0:
                    S.op("vector", lambda e, po=po, tile=tile, cg=cg: e.tensor_copy(out=Yacc[:, tile, cg * 512:(cg + 1) * 512], in_=po),
                         reads=[f"bank{pb}"], writes=[("Y", tile, cg)])
                else:
                    S.op("vector", lambda e, po=po, tile=tile, cg=cg: e.tensor_tensor(
                        out=Yacc[:, tile, cg * 512:(cg + 1) * 512], in0=po, in1=Yacc[:, tile, cg * 512:(cg + 1) * 512], op=ALU.add),
                        reads=[f"bank{pb}", ("Y", tile, cg)], writes=[("Y", tile, cg)])
    S.barrier()

    A.off = 32 * KB
    _gt = A.alloc([64, NOWN], F32)
    _y = A.alloc([128, 8, D], F32)
    gtgF_rep = A.alloc([128, D], F32)
    diagt2 = A.alloc([128, 128], F32)
    x1b = [A.alloc([128, D], F32) for _ in range(8)]
    fjunk = [A.alloc([128, D], BF16) for _ in range(2)]
    build_rep(gtg_f, "gtg_f", gtgF_rep, "gtgF_rep", diagt2)
    finals = []
    for tile in range(8):
        S.op("sync", lambda e, xb=x1b[tile], tile=tile: e.dma_start(out=xb, in_=X1[tile * 128:(tile + 1) * 128, :]),
             writes=[("x1b", tile)], dma=True)
    for tile in range(8):
        xb = x1b[tile]
        xk = ("x1b", tile)
        ss, ssk = sm()
        rs, rsk = sm()
        S.op("scalar", lambda e, tile=tile, ss=ss: e.activation(out=fjunk[tile % 2], in_=Yacc[:, tile, :], func=AF.Square, accum_out=ss),
             writes=[("fjunk", tile % 2), ssk])
        rstd_from_ss(ss, ssk, D, rs, rsk)
        S.op("vector", lambda e, tile=tile, rs=rs: e.scalar_tensor_tensor(out=Yacc[:, tile, :], in0=Yacc[:, tile, :], scalar=rs,
                                                                           in1=gtgF_rep, op0=ALU.mult, op1=ALU.mult),
             reads=[rsk, "gtgF_rep"], writes=[("Yf", tile)])
        S.op("gpsimd", lambda e, tile=tile, xb=xb: e.tensor_tensor(out=xb, in0=xb, in1=Yacc[:, tile, :], op=ALU.add),
             reads=[("Yf", tile), xk], writes=[xk])
        finals.append(S.op("sync", lambda e, tile=tile, xb=xb: e.dma_start(out=out[tile * 128:(tile + 1) * 128, :], in_=xb),
                           reads=[xk], writes=[("out", tile)], dma=True))
    run_sched(nc, S, finals + dbg_ops)
    return nc


def _slot_blocks(c):
    return [8 * s + c if s % 2 == 0 else 8 * s + 7 - c for s in range(8)]


def _rope_tables():
    pos = np.arange(SEQ, dtype=np.float32)
    inv_k = (np.float32(THETA) ** (-np.arange(0, 64, 2, dtype=np.float32) / np.float32(64))).astype(np.float32)
    ang = (pos[:, None] * inv_k[None, :]).astype(np.float32)
    cos, sin = np.cos(ang).astype(np.float32), np.sin(ang).astype(np.float32)
    ck = np.concatenate([cos, cos], axis=1).T
    sk = np.concatenate([-sin, sin], axis=1).T
    ropeK = np.ascontiguousarray(np.stack([ck, sk]).astype(np.float32))
    inv_d = (np.float32(THETA) ** (-np.arange(0, 16, 2, dtype=np.float32) / np.float32(16))).astype(np.float32)
    angd = (pos[:, None] * inv_d[None, :]).astype(np.float32)
    cd, sd = np.cos(angd).astype(np.float32), np.sin(angd).astype(np.float32)
    c64 = np.ones((SEQ, 64), np.float32)
    s64 = np.zeros((SEQ, 64), np.float32)
    c64[:, 0:8] = cd
    c64[:, 8:16] = cd
    s64[:, 0:8] = -sd
    s64[:, 8:16] = sd
    cD = np.concatenate([c64, c64], axis=1).T
    sD = np.concatenate([s64, s64], axis=1).T
    ropeD = np.ascontiguousarray(np.stack([cD, sD]).astype(np.float32))
    return ropeK, ropeD


def _perms():
    P = np.zeros((128, 192), np.float32)
    for m in range(128):
        d = m % 64
        if d < 8:
            P[m + 8, m] = 1.0
        elif d < 16:
            P[m - 8, m] = 1.0
    for m in range(64):
        P[(m + 32) % 64, 128 + m] = 1.0
    return P.astype(ml_dtypes.bfloat16)


def _masks(c):
    diag = np.ones((128, 128), np.float32)
    diag[64:, :64] = 0.0
    M = np.zeros((128, 2, 8, 128), np.float32)
    for par in range(2):
        jd = c if par == 0 else 7 - c
        for j in range(8):
            if j < jd:
                M[:, par, j, :] = 1.0
            elif j == jd:
                M[:, par, j, :] = diag
    return M.astype(ml_dtypes.bfloat16)


_NC_CACHE = {}


def _col(v, n):
    return np.ascontiguousarray(np.asarray(v, np.float32).reshape(n, 128).T)


def make_in_maps(inputs):
    f = lambda k: np.asarray(inputs[k], np.float32)
    x = f("x")[0]
    ropeK, ropeD = _rope_tables()
    perms = _perms()
    shared = {
        "x_all": x,
        "c_col": _col(f("c")[0], 16),
        "w_ada": f("w_ada")[0],
        "b_ada_col": _col(f("b_ada")[0], 96),
        "gcols": np.ascontiguousarray(np.concatenate([_col(f(k)[0], 16) for k in ("g_pre_mix", "g_post_mix", "g_pre_ffn", "g_post_ffn")], axis=1)),
        "gq_col": _col(f("g_q_lat")[0], 6),
        "gkv_col": _col(f("g_kv_lat")[0], 4),
        "lam_row": np.ascontiguousarray(np.concatenate([f(k)[0] for k in ("lambda_q1", "lambda_k1", "lambda_q2", "lambda_k2")])[None, :]),
        "gsub_row": f("g_diff_sub"),
        "gsub_col": np.ascontiguousarray(f("g_diff_sub")[0][:, None]),
        "brt_row": f("b_router"),
        "w_in": f("w_in")[0], "w_uq": f("w_uq")[0], "w_ukv": f("w_ukv")[0], "w_out": f("w_out")[0],
        "w_router": f("w_router")[0], "w_gate": f("w_gate")[0], "w_up": f("w_up")[0], "w_down": f("w_down")[0],
        "ws_gate": f("ws_gate")[0], "ws_up": f("ws_up")[0], "ws_down": f("ws_down")[0],
        "ropeK": ropeK, "ropeD": ropeD, "perms": perms,
    }
    maps = []
    for c in range(NCORES):
        rows = np.concatenate([np.arange(b * 128, (b + 1) * 128) for b in _slot_blocks(c)])
        m = dict(shared)
        m["x_own"] = np.ascontiguousarray(x[rows])
        m["ropeKq"] = np.ascontiguousarray(ropeK[:, :, rows])
        m["ropeDq"] = np.ascontiguousarray(ropeD[:, :, rows])
        m["masks"] = _masks(c)
        maps.append(m)
    return maps


def kernel(**inputs):
    if "nc" not in _NC_CACHE:
        _NC_CACHE["nc"] = build_program(False)
    nc = _NC_CACHE["nc"]
    maps = make_in_maps(inputs)
    res = run_bass_kernel_spmd(nc, maps, core_ids=list(range(NCORES)))
    outp = np.zeros((1, SEQ, D), np.float32)
    for c in range(NCORES):
        o = np.asarray(res.results[c]["out"], np.float32)
        for s, b in enumerate(_slot_blocks(c)):
            outp[0, b * 128:(b + 1) * 128, :] = o[s * 128:(s + 1) * 128, :]
    return outp
```

```python
import contextlib
import math
import numpy as np
import ml_dtypes
import concourse.bass as bass
import concourse.mybir as mybir
from concourse.bass_utils import run_bass_kernel_spmd

F32 = mybir.dt.float32
BF16 = mybir.dt.bfloat16
AF = mybir.ActivationFunctionType
ALU = mybir.AluOpType

ENGS = ("sync", "tensor", "vector", "scalar", "gpsimd")
SEM_WRAP = 30000
NCORES = 8
D = 2048
SEQ = 8192
NOWN = 1024
EPS = 1e-6
THETA = 500000.0
NEXP = 64


class Op:
    __slots__ = ("eng", "fn", "deps", "signal", "dma", "sem", "val")

    def __init__(self, eng, fn, dma):
        self.eng, self.fn, self.dma = eng, fn, dma
        self.deps = []
        self.signal = False
        self.sem = None
        self.val = 0


class Sched:
    def __init__(self):
        self.ops = {e: [] for e in ENGS}
        self.last_w = {}
        self.readers = {}
        self.pending_barrier = {}
        self.dma_since = []

    def op(self, eng, fn, reads=(), writes=(), dma=False):
        o = Op(eng, fn, dma)
        deps = {}
        bank_r = [r for r in reads if isinstance(r, str) and r.startswith("bank")]
        if bank_r:
            reads = [r for r in reads if r not in bank_r]
            writes = list(writes) + bank_r
        for r in reads:
            w = self.last_w.get(r)
            if w is not None:
                deps[id(w)] = w
        for r in writes:
            w = self.last_w.get(r)
            if w is not None:
                deps[id(w)] = w
            for rd in self.readers.get(r, ()):
                deps[id(rd)] = rd
        for r in reads:
            self.readers.setdefault(r, []).append(o)
        for r in writes:
            self.last_w[r] = o
            self.readers[r] = []
        pb = self.pending_barrier.pop(eng, None)
        if pb:
            for d in pb:
                deps[id(d)] = d
        o.deps = [d for d in deps.values() if d is not o]
        for d in o.deps:
            d.signal = True
        self.ops[eng].append(o)
        if dma:
            self.dma_since.append(o)
        return o

    def barrier(self):
        pts = [self.ops[e][-1] for e in ENGS if self.ops[e]] + self.dma_since
        self.dma_since = []
        for e in ENGS:
            self.pending_barrier[e] = list(self.pending_barrier.get(e, [])) + pts
        self.last_w = {}
        self.readers = {}


def run_sched(nc, sched, final_ops, n_dma_sems=16):
    for o in final_ops:
        o.signal = True
    es = contextlib.ExitStack()
    with es:
        eng_sems = {}
        for e in ENGS:
            n_sig = sum(1 for o in sched.ops[e] if o.signal and not o.dma)
            nsem = max(1, (n_sig + SEM_WRAP - 1) // SEM_WRAP)
            eng_sems[e] = [es.enter_context(nc.semaphore(f"s_{e}_{i}")) for i in range(nsem)]
        dma_pool = {e: [es.enter_context(nc.semaphore(f"d_{e}_{i}")) for i in range(n_dma_sems)]
                    for e in ENGS if any(o.dma for o in sched.ops[e])}
        for e in ENGS:
            cnt = 0
            k = 0
            slot_uses = [0] * n_dma_sems
            slot_prev = [None] * n_dma_sems
            di = 0
            for o in sched.ops[e]:
                if o.dma:
                    s = di % n_dma_sems
                    di += 1
                    slot_uses[s] += 1
                    o.sem = dma_pool[e][s]
                    o.val = 16 * slot_uses[s]
                    if slot_prev[s] is not None:
                        o.deps.append(slot_prev[s])
                    slot_prev[s] = o
                    o.signal = True
                elif o.signal:
                    if cnt >= SEM_WRAP:
                        k += 1
                        cnt = 0
                    cnt += 1
                    o.sem = eng_sems[e][k]
                    o.val = cnt
        blk = es.enter_context(nc.Block())

        def make(e):
            def body(eng):
                waited = {}
                for o in sched.ops[e]:
                    for d in o.deps:
                        key = id(d.sem)
                        if waited.get(key, 0) >= d.val:
                            continue
                        eng.wait_ge(d.sem, d.val)
                        waited[key] = d.val
                    ins = o.fn(eng)
                    if o.signal:
                        ins.then_inc(o.sem, 16 if o.dma else 1)
                if e == "sync":
                    for o in final_ops:
                        if waited.get(id(o.sem), 0) < o.val:
                            eng.wait_ge(o.sem, o.val)
                            waited[id(o.sem)] = o.val
            return body

        for e in ENGS:
            getattr(blk, e)(make(e))


class Arena:
    def __init__(self, nc, nbytes):
        self.t = nc.alloc_sbuf_tensor("arena", [128, nbytes // 2], BF16)
        self.cap = nbytes
        self.off = 0

    def alloc(self, shape, dtype):
        esz = 4 if dtype == F32 else 2
        n = int(np.prod(shape[1:]))
        nbytes = (n * esz + 31) // 32 * 32
        assert self.off + nbytes <= self.cap, (self.off, nbytes, self.cap)
        ap = self.t[0:shape[0], self.off // 2:(self.off + n * esz) // 2]
        self.off += nbytes
        if dtype == F32:
            ap = ap.bitcast(F32)
        if len(shape) == 3:
            ap = ap.rearrange("p (a b) -> p a b", a=shape[1])
        elif len(shape) == 4:
            ap = ap.rearrange("p (a b c) -> p a b c", a=shape[1], b=shape[2])
        return ap


def build_program(dbg=False, stop=None, ng1a=None):
    nc = bass.Bass("TRN2", target_bir_lowering=False)
    S = Sched()

    def din(name, shape, dt=F32):
        return nc.dram_tensor(name, list(shape), dt, kind="ExternalInput").ap()

    x_all = din("x_all", [SEQ, D])
    x_own = din("x_own", [NOWN, D])
    c_col = din("c_col", [128, 16])
    w_ada = din("w_ada", [D, 6 * D])
    b_ada_col = din("b_ada_col", [128, 96])
    gcols = din("gcols", [128, 64])
    gq_col = din("gq_col", [128, 6])
    gkv_col = din("gkv_col", [128, 4])
    lam_row = din("lam_row", [1, 256])
    gsub_row = din("gsub_row", [1, 128])
    gsub_col = din("gsub_col", [128, 1])
    brt_row = din("brt_row", [1, 64])
    w_in = din("w_in", [D, 4416])
    w_uq = din("w_uq", [768, 1536])
    w_ukv = din("w_ukv", [512, 2048])
    w_out = din("w_out", [D, D])
    w_router = din("w_router", [D, 64])
    if stop is None:
        w_gate = din("w_gate", [NEXP, D, 512])
        w_up = din("w_up", [NEXP, D, 512])
        w_down = din("w_down", [NEXP, 512, D])
        ws_gate = din("ws_gate", [D, 512])
        ws_up = din("ws_up", [D, 512])
        ws_down = din("ws_down", [512, D])
    ropeK = din("ropeK", [2, 64, SEQ])
    ropeD = din("ropeD", [2, 128, SEQ])
    ropeKq = din("ropeKq", [2, 64, NOWN])
    ropeDq = din("ropeDq", [2, 128, NOWN])
    perms = din("perms", [128, 192], BF16)
    masks = din("masks", [128, 2, 8, 128], BF16)
    out = nc.dram_tensor("out", [NOWN, D], F32, kind="ExternalOutput").ap()

    okind = "ExternalOutput" if dbg else "Internal"
    KT = nc.dram_tensor("KT", [8, 128, SEQ], BF16, kind=okind).ap()
    KPE = nc.dram_tensor("KPE", [64, SEQ], BF16, kind=okind).ap()
    VM = nc.dram_tensor("VM", [SEQ, 8, 128], BF16, kind=okind).ap()
    DKT = nc.dram_tensor("DKT", [8, 128, SEQ], BF16, kind=okind).ap()
    DV = nc.dram_tensor("DV", [SEQ, 8, 128], BF16, kind=okind).ap()
    X1 = nc.dram_tensor("X1", [NOWN, D], F32, kind=okind).ap()
    if dbg:
        OCd = nc.dram_tensor("OCd", [NOWN, D], BF16, kind="ExternalOutput").ap()
        MODd = nc.dram_tensor("MODd", [128, 96], F32, kind="ExternalOutput").ap()
        GATEd = nc.dram_tensor("GATEd", [128, 8, 64], F32, kind="ExternalOutput").ap()

    def sb(name, shape, dt):
        return nc.alloc_sbuf_tensor(name, list(shape), dt)

    ident_f = sb("ident_f", [128, 128], F32)
    ident_b = sb("ident_b", [128, 128], BF16)
    ones_f = sb("ones_f", [128, 128], F32)
    ones_b = sb("ones_b", [128, 128], BF16)
    perm_sb = sb("perm_sb", [128, 192], BF16)
    mask_sb = sb("mask_sb", [128, 2, 8, 128], BF16)
    modc = sb("modc", [128, 96], F32)
    cols = sb("cols", [128, 64], F32)
    gq_sb = sb("gq_sb", [128, 6], F32)
    gkv_sb = sb("gkv_sb", [128, 4], F32)
    gsc_a = sb("gsc_a", [128, 16], F32)
    gsc_f = sb("gsc_f", [128, 16], F32)
    gtg_a = sb("gtg_a", [128, 16], F32)
    gtg_f = sb("gtg_f", [128, 16], F32)
    eps_t = sb("eps_t", [128, 1], F32)
    lamt = sb("lamt", [128, 8], F32)
    lam_in = sb("lam_in", [128, 256], F32)
    lam_junk = sb("lam_junk", [128, 64], F32)
    gsub_sb = sb("gsub_sb", [128, 128], F32)
    brt_sb = sb("brt_sb", [128, 64], F32)
    gsubc = sb("gsubc", [128, 1], F32)
    csil = sb("csil", [128, 16], F32)
    GATE = sb("GATE", [128, 8, 64], F32)
    small = sb("small", [128, 64], F32)

    ARENA_BYTES = 196 * 1024
    A = Arena(nc, ARENA_BYTES)

    banks = [nc.alloc_psum_tensor(f"bank{i}", [128, 512], F32) for i in range(8)]

    def bank_bf(i):
        return banks[i][:].bitcast(BF16)

    sm_ctr = [0]

    def sm():
        i = sm_ctr[0] % 64
        sm_ctr[0] += 1
        return small[:, i:i + 1], ("small", i)

    S.op("gpsimd", lambda e: e.memset(ident_f[:], 0.0), writes=["ident_f"])
    S.op("gpsimd", lambda e: e.affine_select(out=ident_f[:], in_=ident_f[:], compare_op=ALU.not_equal,
                                              fill=1.0, base=0, pattern=[[-1, 128]], channel_multiplier=1),
         reads=["ident_f"], writes=["ident_f"])
    S.op("vector", lambda e: e.tensor_copy(out=ident_b[:], in_=ident_f[:]), reads=["ident_f"], writes=["ident_b"])
    S.op("gpsimd", lambda e: e.memset(ones_f[:], 1.0), writes=["ones_f"])
    S.op("gpsimd", lambda e: e.memset(ones_b[:], 1.0), writes=["ones_b"])
    S.op("gpsimd", lambda e: e.memset(eps_t[:], EPS), writes=["eps"])
    for (dst, src, key) in ((perm_sb[:], perms, "perm"), (mask_sb[:], masks, "mask"), (cols[:], gcols, "cols"),
                            (gq_sb[:], gq_col, "gq"), (gsubc[:], gsub_col, "gsubc"), (gkv_sb[:], gkv_col, "gkv"), (csil[:], c_col, "csil"),
                            (modc[:], b_ada_col, "modb"),
                            (lam_in[:], lam_row.broadcast_to([128, 256]), "lam_in"),
                            (gsub_sb[:], gsub_row.broadcast_to([128, 128]), "gsub"),
                            (brt_sb[:], brt_row.broadcast_to([128, 64]), "brt")):
        S.op("sync", (lambda e, d=dst, s_=src: e.dma_start(out=d, in_=s_)), writes=[key], dma=True)
    S.op("scalar", lambda e: e.activation(out=csil[:], in_=csil[:], func=AF.Silu), reads=["csil"], writes=["csil"])

    lambda_init = 0.8 - 0.6 * math.exp(-0.3 * 0)
    S.op("vector", lambda e: e.tensor_tensor(out=lam_junk[:], in0=lam_in[:, 0:64], in1=lam_in[:, 64:128], op=ALU.mult),
         reads=["lam_in"], writes=["lam_junk"])
    S.op("vector", lambda e: e.reduce_sum(out=lamt[:, 0:1], in_=lam_junk[:], axis=mybir.AxisListType.X),
         reads=["lam_junk"], writes=["lam0"])
    S.op("vector", lambda e: e.tensor_tensor(out=lam_junk[:], in0=lam_in[:, 128:192], in1=lam_in[:, 192:256], op=ALU.mult),
         reads=["lam_in", "lam0"], writes=["lam_junk"])
    S.op("vector", lambda e: e.reduce_sum(out=lamt[:, 1:2], in_=lam_junk[:], axis=mybir.AxisListType.X),
         reads=["lam_junk"], writes=["lam1"])
    S.op("scalar", lambda e: e.activation(out=lamt[:, 2:4], in_=lamt[:, 0:2], func=AF.Exp), reads=["lam0", "lam1"], writes=["lam2"])
    S.op("vector", lambda e: e.tensor_tensor(out=lamt[:, 4:5], in0=lamt[:, 2:3], in1=lamt[:, 3:4], op=ALU.subtract),
         reads=["lam2"], writes=["lam4"])
    S.op("vector", lambda e: e.tensor_scalar(out=lamt[:, 5:6], in0=lamt[:, 4:5], scalar1=lambda_init, scalar2=-1.0,
                                             op0=ALU.add, op1=ALU.mult), reads=["lam4"], writes=["neglam"])
    S.op("vector", lambda e: e.tensor_scalar(out=gsub_sb[:], in0=gsub_sb[:], scalar1=1.0 - lambda_init, scalar2=None,
                                             op0=ALU.mult), reads=["gsub"], writes=["gsub"])
    S.op("vector", lambda e: e.tensor_scalar(out=gsubc[:], in0=gsubc[:], scalar1=1.0 - lambda_init, scalar2=None,
                                             op0=ALU.mult), reads=["gsubc"], writes=["gsubc"])

    wKV = A.alloc([128, 16, 2624], BF16)
    wukv = A.alloc([128, 4, 2048], BF16)
    p1a_mark = A.off
    w_in_v = w_in.rearrange("(kc p) n -> p kc n", p=128)
    for kc in range(16):
        S.op("gpsimd", lambda e, kc=kc: e.dma_start(out=wKV[:, kc, 0:576], in_=w_in_v[:, kc, 768:1344]),
             writes=[("wKV", kc)], dma=True)
        S.op("gpsimd", lambda e, kc=kc: e.dma_start(out=wKV[:, kc, 576:2624], in_=w_in_v[:, kc, 2368:4416]),
             writes=[("wKV", kc, 1)], dma=True)
    S.op("gpsimd", lambda e: e.dma_start(out=wukv, in_=w_ukv.rearrange("(kc p) n -> p kc n", p=128)),
         writes=["wukv"], dma=True)
    acc_ada = A.alloc([128, 6 * D], F32)
    NWB = 3
    wa = [A.alloc([128, 4096], F32) for _ in range(NWB)]
    pmod = banks[0][:, 0:96]
    ci = 0
    for kc in range(16):
        for th in range(3):
            b = ci % NWB
            ci += 1
            S.op("sync" if ci % 2 == 0 else "scalar",
                 (lambda e, kc=kc, b=b, th=th: e.dma_start(out=wa[b], in_=w_ada[kc * 128:(kc + 1) * 128, th * 4096:(th + 1) * 4096])),
                 writes=[("wa", b)], dma=True)
            if kc == 0:
                S.op("vector", lambda e, b=b, th=th: e.tensor_scalar(out=acc_ada[:, th * 4096:(th + 1) * 4096], in0=wa[b], scalar1=csil[:, 0:1],
                                                                      scalar2=None, op0=ALU.mult),
                     reads=[("wa", b), "csil"], writes=[("acc", th)])
            else:
                S.op("vector", lambda e, b=b, th=th, kc=kc: e.scalar_tensor_tensor(
                    out=acc_ada[:, th * 4096:(th + 1) * 4096], in0=wa[b], scalar=csil[:, kc:kc + 1], in1=acc_ada[:, th * 4096:(th + 1) * 4096],
                    op0=ALU.mult, op1=ALU.add), reads=[("wa", b), "csil", ("acc", th)], writes=[("acc", th)])

    def mm_ada(e):
        ins = None
        for j in range(96):
            ins = e.matmul(pmod[:, j:j + 1], lhsT=acc_ada[:, j * 128:(j + 1) * 128], rhs=ones_f[:, 0:1],
                           start=True, stop=True, skip_group_check=True)
        return ins
    S.op("tensor", mm_ada, reads=[("acc", 0), ("acc", 1), ("acc", 2), "ones_f"], writes=["bank0"])
    S.op("vector", lambda e: e.tensor_tensor(out=modc[:], in0=pmod, in1=modc[:], op=ALU.add),
         reads=["bank0", "modb"], writes=["modc"])
    dbg_ops = []
    if dbg:
        dbg_ops.append(S.op("sync", lambda e: e.dma_start(out=MODd, in_=modc[:]), reads=["modc"], dma=True))
    S.op("vector", lambda e: e.scalar_tensor_tensor(out=gsc_a[:], in0=modc[:, 16:32], scalar=1.0, in1=cols[:, 0:16],
                                                    op0=ALU.add, op1=ALU.mult), reads=["modc", "cols"], writes=["gsc_a"])
    S.op("vector", lambda e: e.scalar_tensor_tensor(out=gsc_f[:], in0=modc[:, 64:80], scalar=1.0, in1=cols[:, 32:48],
                                                    op0=ALU.add, op1=ALU.mult), reads=["modc", "cols"], writes=["gsc_f"])
    S.op("vector", lambda e: e.tensor_tensor(out=gtg_a[:], in0=modc[:, 32:48], in1=cols[:, 16:32], op=ALU.mult),
         reads=["modc", "cols"], writes=["gtg_a"])
    S.op("vector", lambda e: e.tensor_tensor(out=gtg_f[:], in0=modc[:, 80:96], in1=cols[:, 48:64], op=ALU.mult),
         reads=["modc", "cols"], writes=["gtg_f"])
    S.barrier()
    A.off = 0
    if stop == "p0":
        run_sched(nc, S, dbg_ops)
        return nc

    def rstd_from_ss(ss_ap, ss_key, n, out_ap, out_key):
        S.op("scalar", lambda e: e.activation(out=out_ap, in_=ss_ap, func=AF.Ln, scale=1.0 / n, bias=eps_t[0:ss_ap.shape[0], 0:1]),
             reads=[ss_key, "eps"], writes=[out_key])
        S.op("scalar", lambda e: e.activation(out=out_ap, in_=out_ap, func=AF.Exp, scale=-0.5),
             reads=[out_key], writes=[out_key])

    def load_norm_transpose(src_rows, hT_ap, hT_key, col0, xs, xs_key, xn, xn_key, pbanks, gsc, sh_lo, junk, junk_key):
        S.op("sync", lambda e: e.dma_start(out=xs, in_=src_rows), writes=[xs_key], dma=True)
        ss, ssk = sm()
        rs, rsk = sm()
        S.op("scalar", lambda e: e.activation(out=junk, in_=xs, func=AF.Square, accum_out=ss),
             reads=[xs_key], writes=[junk_key, ssk])
        rstd_from_ss(ss, ssk, D, rs, rsk)
        S.op("vector", lambda e: e.tensor_scalar(out=xn, in0=xs, scalar1=rs, scalar2=None, op0=ALU.mult),
             reads=[xs_key, rsk], writes=[xn_key])
        for hb in range(2):
            pb = pbanks[hb]
            pv = bank_bf(pb)[:, 0:1024].rearrange("p (a b) -> p a b", a=8)

            def tr(e, hb=hb, pv=pv):
                ins = None
                for j in range(8):
                    kc = hb * 8 + j
                    ins = e.transpose(out=pv[:, j, :], in_=xn[:, kc * 128:(kc + 1) * 128], identity=ident_b[:])
                return ins
            S.op("tensor", tr, reads=[xn_key, "ident_b"], writes=[f"bank{pb}"])

            def ev(e, hb=hb, pv=pv):
                ins = None
                for j in range(8):
                    kc = hb * 8 + j
                    ins = e.tensor_scalar(out=hT_ap[:, kc, col0:col0 + 128], in0=pv[:, j, :],
                                          scalar1=gsc[:, kc:kc + 1], scalar2=modc[:, sh_lo + kc:sh_lo + kc + 1],
                                          op0=ALU.mult, op1=ALU.add)
                return ins
            S.op("vector", ev, reads=[f"bank{pb}", "gsc_a", "gsc_f", "modc"], writes=[hT_key])

    def mm_acc(out_ap, pairs, reads, writes):
        def f(e):
            ins = None
            n = len(pairs)
            for i, (l, r) in enumerate(pairs):
                ins = e.matmul(out_ap, lhsT=l, rhs=r, start=(i == 0), stop=(i == n - 1))
            return ins
        return S.op("tensor", f, reads=reads, writes=writes)

    NG = 256
    A.off = p1a_mark
    wKV_keys = [("wKV", kc) for kc in range(16)] + [("wKV", kc, 1) for kc in range(16)]

    xs2 = [A.alloc([128, D], F32) for _ in range(2)]
    junkA = A.alloc([128, D], BF16)
    hT2 = [A.alloc([128, 16, NG], BF16) for _ in range(2)]
    kvraw = A.alloc([128, 4, NG], F32)
    sqb = A.alloc([128, 4, NG], BF16)
    rrep = A.alloc([128, NG], F32)
    kvn = A.alloc([128, 4, NG], BF16)
    kT_out = A.alloc([128, 8, NG], BF16)
    v_out = A.alloc([128, 2, 1024], BF16)
    kpe_bf = A.alloc([64, NG], BF16)
    kpe_out = A.alloc([64, NG], BF16)
    dk_bf = A.alloc([128, 8, NG], BF16)
    dkT_out = A.alloc([128, 8, NG], BF16)
    dv_out = A.alloc([128, 2, 1024], BF16)
    tabK = A.alloc([64, 2, NG], F32)
    tabD = A.alloc([128, 2, NG], F32)
    t1 = A.alloc([128, NG], F32)
    t2 = A.alloc([128, NG], F32)
    permD = perm_sb[:, 0:128]
    permK = perm_sb[0:64, 128:192]

    def rope_apply(src_bf, src_key, ptmp_bank, perm, tab, tab_key, dst, dst_key, np_, t1, t2):
        pt = banks[ptmp_bank][0:np_, 0:NG]
        S.op("tensor", lambda e: e.matmul(pt, lhsT=perm, rhs=src_bf, start=True, stop=True),
             reads=[src_key, "perm"], writes=[f"bank{ptmp_bank}"])
        S.op("vector", lambda e: e.tensor_tensor(out=t2[0:np_, :], in0=pt, in1=tab[:, 1, :], op=ALU.mult),
             reads=[f"bank{ptmp_bank}", tab_key], writes=["t2"])
        S.op("gpsimd", lambda e: e.tensor_tensor(out=t1[0:np_, :], in0=src_bf, in1=tab[:, 0, :], op=ALU.mult),
             reads=[src_key, tab_key], writes=["t1"])
        S.op("vector", lambda e: e.tensor_tensor(out=dst, in0=t1[0:np_, :], in1=t2[0:np_, :], op=ALU.add),
             reads=["t1", "t2"], writes=[dst_key])

    rot = {}

    def next_bank(lst):
        k = tuple(lst)
        i = rot.get(k, 0)
        rot[k] = i + 1
        return lst[i % len(lst)]

    FM_BANKS = [2, 3, 4]
    TM_BANKS = [5, 6]
    kv_finals = []
    n_g1a = ng1a if ng1a else SEQ // NG
    xn4 = [A.alloc([128, D], BF16) for _ in range(4)]

    def prepA_dma(g, src, xs_l, tag):
        for t in range(2):
            xs = xs_l[t][:, :]
            rows = src[g * NG + t * 128: g * NG + (t + 1) * 128, :]
            S.op("sync", lambda e, xs=xs, rows=rows: e.dma_start(out=xs, in_=rows), writes=[(tag + "xs", t)], dma=True)

    def prepA(g, src, xs_l, xn_l, junk, tag):
        for t in range(2):
            xi = (2 * g + t) % len(xn_l)
            xs, xn = xs_l[t][:, :], xn_l[xi][:, :]
            xs_key, xn_key = (tag + "xs", t), (tag + "xn", xi)
            ss, ssk = sm()
            rs, rsk = sm()
            S.op("scalar", lambda e, xs=xs, ss=ss, junk=junk: e.activation(out=junk, in_=xs, func=AF.Square, accum_out=ss),
                 reads=[xs_key], writes=[tag + "junk", ssk])
            rstd_from_ss(ss, ssk, D, rs, rsk)
            S.op("vector", lambda e, xs=xs, xn=xn, rs=rs: e.tensor_scalar(out=xn, in0=xs, scalar1=rs, scalar2=None, op0=ALU.mult),
                 reads=[xs_key, rsk], writes=[xn_key])

    def prepB(g, hT_l, xn_l, tag, tiles=(0, 1)):
        hT = hT_l[g % 2]
        hkey = (tag + "hT", g % 2)
        for t in tiles:
            xi = (2 * g + t) % len(xn_l)
            xn = xn_l[xi][:, :]
            xn_key = (tag + "xn", xi)
            for hb in range(2):
                pb = hb
                pv = bank_bf(pb)[:, 0:1024].rearrange("p (a b) -> p a b", a=8)

                def tr(e, hb=hb, pv=pv, xn=xn):
                    ins = None
                    for j in range(8):
                        kc = hb * 8 + j
                        ins = e.transpose(out=pv[:, j, :], in_=xn[:, kc * 128:(kc + 1) * 128], identity=ident_b[:])
                    return ins
                S.op("tensor", tr, reads=[xn_key, "ident_b"], writes=[f"bank{pb}"])

                def ev(e, hb=hb, pv=pv, hT=hT, t=t):
                    ins = None
                    for j in range(8):
                        kc = hb * 8 + j
                        ins = e.tensor_scalar(out=hT[:, kc, t * 128:(t + 1) * 128], in0=pv[:, j, :],
                                              scalar1=gsc_a[:, kc:kc + 1], scalar2=modc[:, kc:kc + 1],
                                              op0=ALU.mult, op1=ALU.add)
                    return ins
                S.op("vector", ev, reads=[f"bank{pb}", "gsc_a", "modc"], writes=[hkey])

    prepA_dma(0, x_all, xs2, "a")
    prepA(0, x_all, xs2, xn4, junkA[:, :], "a")
    prepB(0, hT2, xn4, "a")
    if n_g1a > 1:
        prepA_dma(1, x_all, xs2, "a")
        prepA(1, x_all, xs2, xn4, junkA[:, :], "a")
    for g in range(n_g1a):
        hb_i = g % 2
        hT = hT2[hb_i]
        hkey = ("ahT", hb_i)
        tok0 = g * NG
        if g + 2 < n_g1a:
            prepA_dma(g + 2, x_all, xs2, "a")
        S.op("sync", lambda e, tok0=tok0: e.dma_start(out=tabK, in_=ropeK[:, :, tok0:tok0 + NG].rearrange("a p t -> p a t")),
             writes=["tabK"], dma=True)
        S.op("sync", lambda e, tok0=tok0: e.dma_start(out=tabD, in_=ropeD[:, :, tok0:tok0 + NG].rearrange("a p t -> p a t")),
             writes=["tabD"], dma=True)
        for c4 in range(4):
            pb = next_bank(FM_BANKS)
            po = banks[pb][:, 0:NG]
            mm_acc(po, [(wKV[:, kc, c4 * 128:(c4 + 1) * 128], hT[:, kc, :]) for kc in range(16)],
                   reads=[hkey] + wKV_keys, writes=[f"bank{pb}"])
            S.op("scalar", lambda e, po=po, c4=c4: e.activation(out=sqb[:, c4, :], in_=po, func=AF.Square),
                 reads=[f"bank{pb}"], writes=[("sqb", c4)])
            S.op("vector", lambda e, po=po, c4=c4: e.tensor_copy(out=kvraw[:, c4, :], in_=po),
                 reads=[f"bank{pb}"], writes=[("kvraw", c4)])
        pb = next_bank(FM_BANKS)
        po = banks[pb][0:64, 0:NG]
        mm_acc(po, [(wKV[:, kc, 512:576], hT[:, kc, :]) for kc in range(16)], reads=[hkey] + wKV_keys, writes=[f"bank{pb}"])
        S.op("scalar", lambda e, po=po: e.copy(out=kpe_bf, in_=po), reads=[f"bank{pb}"], writes=["kpe_bf"])

        def dk_mm(h):
            pb = next_bank(FM_BANKS)
            po = banks[pb][:, 0:NG]
            mm_acc(po, [(wKV[:, kc, 576 + h * 128:576 + (h + 1) * 128], hT[:, kc, :]) for kc in range(16)],
                   reads=[hkey] + wKV_keys, writes=[f"bank{pb}"])
            S.op("scalar", lambda e, po=po, h=h: e.copy(out=dk_bf[:, h, :], in_=po), reads=[f"bank{pb}"], writes=[("dk_bf", h)])

        def dk_rope(h):
            rope_apply(dk_bf[:, h, :], ("dk_bf", h), 7, permD, tabD, "tabD", dkT_out[:, h, :], ("dkT_out", h), 128, t1, t2)
        dk_mm(0)
        pss = banks[7][:, 0:NG]
        mm_acc(pss, [(ones_b[:], sqb[:, c4, :]) for c4 in range(4)],
               reads=[("sqb", c4) for c4 in range(4)] + ["ones_b"], writes=["bank7"])
        S.op("scalar", lambda e, pss=pss: e.activation(out=rrep, in_=pss, func=AF.Ln, scale=1.0 / 512, bias=eps_t[:, 0:1]),
             reads=["bank7", "eps"], writes=["rrep"])
        S.op("scalar", lambda e: e.activation(out=rrep, in_=rrep, func=AF.Exp, scale=-0.5), reads=["rrep"], writes=["rrep"])
        for c4 in range(4):
            S.op("vector", lambda e, c4=c4: e.scalar_tensor_tensor(out=kvn[:, c4, :], in0=kvraw[:, c4, :],
                                                                    scalar=gkv_sb[:, c4:c4 + 1], in1=rrep,
                                                                    op0=ALU.mult, op1=ALU.mult),
                 reads=[("kvraw", c4), "gkv", "rrep"], writes=[("kvn", c4)])
        kvn_keys = [("kvn", c4) for c4 in range(4)]
        rope_apply(kpe_bf, "kpe_bf", 7, permK, tabK, "tabK", kpe_out, "kpe_out", 64, t1, t2)
        kv_finals.append(S.op("sync", lambda e, tok0=tok0: e.dma_start(out=KPE[:, tok0:tok0 + NG], in_=kpe_out), reads=["kpe_out"],
             writes=["KPE"], dma=True))
        for h in range(1, 8):
            dk_mm(h)
            dk_rope(h - 1)
        if g + 2 < n_g1a:
            prepA(g + 2, x_all, xs2, xn4, junkA[:, :], "a")
        if g + 1 < n_g1a:
            prepB(g + 1, hT2, xn4, "a", tiles=(0,))
        first = True
        for t in range(2):
            if t == 1 and g + 1 < n_g1a:
                prepB(g + 1, hT2, xn4, "a", tiles=(1,))
            for cg in range(2):
                pb = next_bank(TM_BANKS)
                po = banks[pb][:, :]
                mm_acc(po, [(hT[:, kc, t * 128:(t + 1) * 128], wKV[:, kc, 1600 + cg * 512:1600 + (cg + 1) * 512]) for kc in range(16)],
                       reads=[hkey] + wKV_keys, writes=[f"bank{pb}"])
                S.op("scalar", lambda e, po=po, t=t, cg=cg: e.copy(out=dv_out[:, t, cg * 512:(cg + 1) * 512], in_=po),
                     reads=[f"bank{pb}"], writes=[("dv_out", t, cg)])
                if first:
                    dk_rope(7)
                    kv_finals.append(S.op("sync", lambda e, tok0=tok0: e.dma_start(out=DKT[:, :, tok0:tok0 + NG].rearrange("h p t -> p h t"), in_=dkT_out),
                         reads=[("dkT_out", h) for h in range(8)], writes=["DKT"], dma=True))
                    first = False
        kv_finals.append(S.op("sync", lambda e, tok0=tok0: e.dma_start(
            out=DV[tok0:tok0 + NG, :, :].rearrange("(t p) h d -> p t (h d)", p=128), in_=dv_out),
            reads=[("dv_out", t, cg) for t in range(2) for cg in range(2)], writes=["DV"], dma=True))
        for h in range(8):
            pb = next_bank(FM_BANKS)
            po = banks[pb][:, 0:NG]
            mm_acc(po, [(wukv[:, c4, h * 256:h * 256 + 128], kvn[:, c4, :]) for c4 in range(4)],
                   reads=kvn_keys + ["wukv"], writes=[f"bank{pb}"])
            S.op("vector", lambda e, po=po, h=h: e.tensor_copy(out=kT_out[:, h, :], in_=po),
                 reads=[f"bank{pb}"], writes=[("kT_out", h)])
        kv_finals.append(S.op("sync", lambda e, tok0=tok0: e.dma_start(out=KT[:, :, tok0:tok0 + NG].rearrange("h p t -> p h t"), in_=kT_out),
             reads=[("kT_out", h) for h in range(8)], writes=["KT"], dma=True))
        wukv_v = wukv.rearrange("p c (h x) -> p c h x", h=8)
        for t in range(2):
            for cg in range(2):
                pb = next_bank(TM_BANKS)
                po = banks[pb][:, :]

                def mmv(e, po=po, t=t, cg=cg):
                    ins = None
                    for hh in range(4):
                        for c4 in range(4):
                            ins = e.matmul(po[:, hh * 128:(hh + 1) * 128], lhsT=kvn[:, c4, t * 128:(t + 1) * 128],
                                           rhs=wukv_v[:, c4, cg * 4 + hh, 128:256], start=(c4 == 0), stop=(c4 == 3))
                    return ins
                S.op("tensor", mmv, reads=kvn_keys + ["wukv"], writes=[f"bank{pb}"])
                S.op("scalar", lambda e, po=po, t=t, cg=cg: e.copy(out=v_out[:, t, cg * 512:(cg + 1) * 512], in_=po),
                     reads=[f"bank{pb}"], writes=[("v_out", t, cg)])
        kv_finals.append(S.op("sync", lambda e, tok0=tok0: e.dma_start(
            out=VM[tok0:tok0 + NG, :, :].rearrange("(t p) h d -> p t (h d)", p=128), in_=v_out),
            reads=[("v_out", t, cg) for t in range(2) for cg in range(2)], writes=["VM"], dma=True))
    S.barrier()
    A.off = 0
    if stop == "p1a":
        run_sched(nc, S, dbg_ops + kv_finals)
        return nc

    KB = 1024
    A.off = 0
    h2T = A.alloc([128, 16, NOWN], BF16)
    OC = A.alloc([128, 8, D], BF16)
    QN = A.alloc([128, 8, NOWN], BF16)
    QPE = A.alloc([64, 8, NOWN], BF16)
    DQ = A.alloc([128, 8, NOWN], BF16)
    assert A.off == 112 * KB
    A.off = 0
    wQ = A.alloc([128, 16, 1792], BF16)
    assert A.off <= 64 * KB
    A.off = 112 * KB
    wuq = A.alloc([128, 6, 1536], BF16)
    for kc in range(16):
        S.op("gpsimd", lambda e, kc=kc: e.dma_start(out=wQ[:, kc, 0:768], in_=w_in_v[:, kc, 0:768]), writes=[("wQ", kc)], dma=True)
        S.op("gpsimd", lambda e, kc=kc: e.dma_start(out=wQ[:, kc, 768:1792], in_=w_in_v[:, kc, 1344:2368]), writes=[("wQ", kc, 1)], dma=True)
    S.op("gpsimd", lambda e: e.dma_start(out=wuq, in_=w_uq.rearrange("(kc p) n -> p kc n", p=128)), writes=["wuq"], dma=True)
    wQ_keys = [("wQ", kc) for kc in range(16)] + [("wQ", kc, 1) for kc in range(16)]
    xs2q = [A.alloc([128, D], F32) for _ in range(2)]
    xn2q = [A.alloc([128, D], BF16) for _ in range(2)]
    junkQb = A.alloc([128, D], BF16)
    hT2q = [A.alloc([128, 16, NG], BF16) for _ in range(2)]
    qraw = A.alloc([128, 6, NG], F32)
    sq6 = A.alloc([128, 6, NG], BF16)
    rrepq_t = A.alloc([128, NG], F32)
    qn = A.alloc([128, 6, NG], BF16)
    qpe_bf = A.alloc([64, NG], BF16)
    dq_bf = A.alloc([128, NG], BF16)
    tabKq_t = A.alloc([64, 2, NG], F32)
    tabDq_t = A.alloc([128, 2, NG], F32)
    t1q = A.alloc([128, NG], F32)
    t2q = A.alloc([128, NG], F32)
    qpe_bf2 = [qpe_bf, A.alloc([64, NG], BF16)]
    dq_bf2 = [dq_bf, A.alloc([128, NG], BF16)]
    _save = A.off
    A.off = 56 * KB
    xn4q = xn2q + [A.alloc([128, D], BF16) for _ in range(2)]
    assert A.off <= 64 * KB
    A.off = _save
    n_gq = NOWN // NG
    prepA_dma(0, x_own, xs2q, "q")
    prepA(0, x_own, xs2q, xn4q, junkQb[:, :], "q")
    prepB(0, hT2q, xn4q, "q")
    prepA_dma(1, x_own, xs2q, "q")
    prepA(1, x_own, xs2q, xn4q, junkQb[:, :], "q")
    for g in range(n_gq):
        hb_i = g % 2
        hT = hT2q[hb_i]
        hkey = ("qhT", hb_i)
        tok0 = g * NG
        if g + 2 < n_gq:
            prepA_dma(g + 2, x_own, xs2q, "q")
        S.op("sync", lambda e, tok0=tok0: e.dma_start(out=tabKq_t, in_=ropeKq[:, :, tok0:tok0 + NG].rearrange("a p t -> p a t")),
             writes=["tabKq"], dma=True)
        S.op("sync", lambda e, tok0=tok0: e.dma_start(out=tabDq_t, in_=ropeDq[:, :, tok0:tok0 + NG].rearrange("a p t -> p a t")),
             writes=["tabDq"], dma=True)
        for c6 in range(6):
            pb = next_bank(FM_BANKS)
            po = banks[pb][:, 0:NG]
            mm_acc(po, [(wQ[:, kc, c6 * 128:(c6 + 1) * 128], hT[:, kc, :]) for kc in range(16)],
                   reads=[hkey] + wQ_keys, writes=[f"bank{pb}"])
            S.op("scalar", lambda e, po=po, c6=c6: e.activation(out=sq6[:, c6, :], in_=po, func=AF.Square),
                 reads=[f"bank{pb}"], writes=[("sq6", c6)])
            S.op("vector", lambda e, po=po, c6=c6: e.tensor_copy(out=qraw[:, c6, :], in_=po),
                 reads=[f"bank{pb}"], writes=[("qraw", c6)])

        def dq_mm(h):
            pb = next_bank(FM_BANKS)
            po = banks[pb][:, 0:NG]
            mm_acc(po, [(wQ[:, kc, 768 + h * 128:768 + (h + 1) * 128], hT[:, kc, :]) for kc in range(16)],
                   reads=[hkey] + wQ_keys, writes=[f"bank{pb}"])
            S.op("scalar", lambda e, po=po, h=h: e.copy(out=dq_bf2[h % 2], in_=po), reads=[f"bank{pb}"], writes=[("dq_bf", h % 2)])

        def dq_rope(h):
            rope_apply(dq_bf2[h % 2], ("dq_bf", h % 2), 7, permD, tabDq_t, "tabDq", DQ[:, h, tok0:tok0 + NG], ("DQ", h, g), 128, t1q, t2q)
        dq_mm(0)
        pss = banks[6][:, 0:NG]
        mm_acc(pss, [(ones_b[:], sq6[:, c6, :]) for c6 in range(6)],
               reads=[("sq6", c6) for c6 in range(6)] + ["ones_b"], writes=["bank6"])
        S.op("scalar", lambda e, pss=pss: e.activation(out=rrepq_t, in_=pss, func=AF.Ln, scale=1.0 / 768, bias=eps_t[:, 0:1]),
             reads=["bank6", "eps"], writes=["rrepq"])
        S.op("scalar", lambda e: e.activation(out=rrepq_t, in_=rrepq_t, func=AF.Exp, scale=-0.5), reads=["rrepq"], writes=["rrepq"])
        for c6 in range(6):
            S.op("vector", lambda e, c6=c6: e.scalar_tensor_tensor(out=qn[:, c6, :], in0=qraw[:, c6, :],
                                                                    scalar=gq_sb[:, c6:c6 + 1], in1=rrepq_t,
                                                                    op0=ALU.mult, op1=ALU.mult),
                 reads=[("qraw", c6), "gq", "rrepq"], writes=[("qn", c6)])
        qn_keys = [("qn", c6) for c6 in range(6)]
        for h in range(1, 8):
            dq_mm(h)
            dq_rope(h - 1)
        if g + 2 < n_gq:
            prepA(g + 2, x_own, xs2q, xn4q, junkQb[:, :], "q")
        if g + 1 < n_gq:
            prepB(g + 1, hT2q, xn4q, "q")

        def qpe_rope(h):
            rope_apply(qpe_bf2[h % 2], ("qpe_bf", h % 2), 7, permK, tabKq_t, "tabKq", QPE[:, h, tok0:tok0 + NG], ("QPE", h, g), 64, t1q, t2q)
        for h in range(8):
            pb = next_bank(FM_BANKS)
            po = banks[pb][:, 0:NG]
            mm_acc(po, [(wuq[:, c6, h * 192:h * 192 + 128], qn[:, c6, :]) for c6 in range(6)],
                   reads=qn_keys + ["wuq"], writes=[f"bank{pb}"])
            S.op("vector", lambda e, po=po, h=h, tok0=tok0: e.tensor_copy(out=QN[:, h, tok0:tok0 + NG], in_=po),
                 reads=[f"bank{pb}"], writes=[("QN", h, g)])
            if h == 0:
                dq_rope(7)
            pb = next_bank(FM_BANKS)
            po = banks[pb][0:64, 0:NG]
            mm_acc(po, [(wuq[:, c6, h * 192 + 128:h * 192 + 192], qn[:, c6, :]) for c6 in range(6)],
                   reads=qn_keys + ["wuq"], writes=[f"bank{pb}"])
            S.op("scalar", lambda e, po=po, h=h: e.copy(out=qpe_bf2[h % 2], in_=po), reads=[f"bank{pb}"], writes=[("qpe_bf", h % 2)])
            if h > 0:
                qpe_rope(h - 1)
        qpe_rope(7)
    S.barrier()
    A.off = 112 * KB

    OCT = OC.rearrange("p s d -> p (s d)").rearrange("p (k t) -> p k t", k=16)
    A.off = 0
    Vb = [A.alloc([128, 64, 128], BF16) for _ in range(2)]
    assert A.off <= 32 * KB
    A.off = 112 * KB
    KTb = [A.alloc([128, SEQ], BF16) for _ in range(2)]
    KPEs = A.alloc([64, SEQ], BF16)
    NPT = 8
    PT = [A.alloc([128, 512], BF16) for _ in range(NPT)]
    Psum2 = [A.alloc([128, NOWN], F32) for _ in range(2)]
    rl = A.alloc([128, NOWN], F32)
    osb0 = A.alloc([128, NOWN], F32)
    otmp = A.alloc([128, NOWN], F32)
    sqd = A.alloc([128, NOWN], BF16)
    rstd_t = A.alloc([128, NOWN], F32)
    S.op("sync", lambda e: e.dma_start(out=KPEs, in_=KPE), writes=["KPEs"], dma=True)

    S_BANKS = [0, 1, 2, 5, 6]
    sc_mla = (128 + 64) ** -0.5
    sc_diff = 64 ** -0.5
    pt_i = [0]

    def load_head(hh):
        is_mla = hh < 8
        h = hh % 8
        kb_i = hh % 2
        Ksrc = KT if is_mla else DKT
        Vsrc = VM if is_mla else DV
        Kt = KTb[kb_i]
        Vt = Vb[kb_i]
        for q4 in range(4):
            S.op("sync", lambda e, Kt=Kt, Ksrc=Ksrc, h=h, q4=q4: e.dma_start(
                out=Kt[:, q4 * 2048:(q4 + 1) * 2048], in_=Ksrc[h, :, q4 * 2048:(q4 + 1) * 2048]),
                writes=[("Kt", kb_i, q4)], dma=True)
            S.op("sync", lambda e, Vt=Vt, Vsrc=Vsrc, h=h, q4=q4: e.dma_start(
                out=Vt[:, q4 * 16:(q4 + 1) * 16, :],
                in_=Vsrc[q4 * 2048:(q4 + 1) * 2048, h, :].rearrange("(b p) d -> p b d", p=128)),
                writes=[("Vt", kb_i, q4)], dma=True)

    def denominators(Psum, pi_):
        for half in range(2):
            pb = 7
            S.op("tensor", lambda e, pb=pb, half=half, Psum=Psum: e.matmul(banks[pb][:, :], lhsT=ones_f[:], rhs=Psum[:, half * 512:(half + 1) * 512],
                                                                 start=True, stop=True),
                 reads=[("Psum", pi_, half), "ones_f"], writes=[f"bank{pb}"])
            S.op("vector", lambda e, pb=pb, half=half: e.reciprocal(out=rl[:, half * 512:(half + 1) * 512], in_=banks[pb][:, :]),
                 reads=[f"bank{pb}"], writes=[("rl", half)])

    def evac(hh, comp, Psum, pi_):
        is_mla = hh < 8
        h = hh % 8
        denominators(Psum, pi_)
        if is_mla:
            for half in range(2):
                S.op("vector", lambda e, half=half, h=h: e.tensor_tensor(
                    out=OCT[:, h, half * 512:(half + 1) * 512], in0=banks[3 + half][:, :], in1=rl[:, half * 512:(half + 1) * 512], op=ALU.mult),
                    reads=[f"bank{3 + half}", ("rl", half)], writes=[("OCT", hh, half)])
        elif comp == 0:
            for half in range(2):
                S.op("vector", lambda e, half=half: e.tensor_tensor(
                    out=osb0[:, half * 512:(half + 1) * 512], in0=banks[3 + half][:, :], in1=rl[:, half * 512:(half + 1) * 512], op=ALU.mult),
                    reads=[f"bank{3 + half}", ("rl", half)], writes=[("osb0", half)])
        else:
            for half in range(2):
                hs = slice(half * 512, (half + 1) * 512)
                S.op("vector", lambda e, half=half, hs=hs: e.tensor_tensor(
                    out=otmp[:, hs], in0=banks[3 + half][:, :], in1=rl[:, hs], op=ALU.mult),
                    reads=[f"bank{3 + half}", ("rl", half)], writes=[("otmp", half)])
                S.op("vector", lambda e, hs=hs: e.scalar_tensor_tensor(out=otmp[:, hs], in0=otmp[:, hs], scalar=lamt[:, 5:6], in1=osb0[:, hs],
                                                                        op0=ALU.mult, op1=ALU.add),
                     reads=[("otmp", half), ("osb0", half), "neglam"], writes=[("otmp", half)])
                S.op("scalar", lambda e, hs=hs: e.activation(out=sqd[:, hs], in_=otmp[:, hs], func=AF.Square),
                     reads=[("otmp", half)], writes=[("sqd", half)])
                pb = 7
                S.op("tensor", lambda e, pb=pb, hs=hs: e.matmul(banks[pb][:, :], lhsT=ones_b[:], rhs=sqd[:, hs], start=True, stop=True),
                     reads=[("sqd", half), "ones_b"], writes=[f"bank{pb}"])
                S.op("scalar", lambda e, pb=pb, hs=hs: e.activation(out=rstd_t[:, hs], in_=banks[pb][:, :], func=AF.Ln, scale=1.0 / 128,
                                                                     bias=eps_t[:, 0:1]),
                     reads=[f"bank{pb}", "eps"], writes=[("rstd_t", half)])
                S.op("scalar", lambda e, hs=hs: e.activation(out=rstd_t[:, hs], in_=rstd_t[:, hs], func=AF.Exp, scale=-0.5),
                     reads=[("rstd_t", half)], writes=[("rstd_t", half)])
                S.op("vector", lambda e, hs=hs, h=h: e.scalar_tensor_tensor(
                    out=OCT[:, 8 + h, hs], in0=otmp[:, hs], scalar=gsubc[:, 0:1], in1=rstd_t[:, hs], op0=ALU.mult, op1=ALU.mult),
                    reads=[("otmp", half), ("rstd_t", half), "gsubc"], writes=[("OCT", hh, half)])

    WARM_EVERY = 4
    warm_i = [0]
    LOOK = 2
    GRP = 2
    pend = []
    dev = [None]
    hc_i = 0
    load_head(0)
    for hh in range(16):
        is_mla = hh < 8
        h = hh % 8
        kb_i = hh % 2
        Kt = KTb[kb_i]
        Vt = Vb[kb_i]
        ncomp = 1 if is_mla else 2
        for comp in range(ncomp):
            pi_ = hc_i % 2
            Psum = Psum2[pi_]
            hc_i += 1
            first_o = {3: True, 4: True}
            clist = []
            for kb in range(64):
                q0 = (kb // 8) * 128
                if q0 < 512:
                    clist.append((kb, q0, 512, q0))
                clist.append((kb, max(q0, 512), 1024, q0))
            nsc = 0
            for g0 in range(0, len(clist), GRP):
                grp = []
                for (kb, qa, qe, q0) in clist[g0:g0 + GRP]:
                    n = qe - qa
                    sb_ = next_bank(S_BANKS)
                    pi = pt_i[0] % NPT
                    pt_i[0] += 1
                    obank = 3 + qa // 512
                    st = first_o[obank]
                    first_o[obank] = False
                    grp.append(dict(kb=kb, qa=qa, qe=qe, q0=q0, n=n, sb=sb_, sbank=banks[sb_][:, 0:n], pi=pi, pt=PT[pi][:, 0:n],
                                    ptk=("PT", pi), obank=obank, st=st, q4=kb // 16, half=qa // 512))

                def smm(e, grp=grp, Kt=Kt, h=h, comp=comp, is_mla=is_mla):
                    ins = None
                    for c in grp:
                        kb, qa, qe, sbank = c["kb"], c["qa"], c["qe"], c["sbank"]
                        if is_mla:
                            e.matmul(sbank, lhsT=Kt[:, kb * 128:(kb + 1) * 128], rhs=QN[:, h, qa:qe], start=True, stop=False)
                            ins = e.matmul(sbank, lhsT=KPEs[:, kb * 128:(kb + 1) * 128], rhs=QPE[:, h, qa:qe], start=False, stop=True)
                        else:
                            lo = comp * 64
                            ins = e.matmul(sbank, lhsT=Kt[lo:lo + 64, kb * 128:(kb + 1) * 128], rhs=DQ[lo:lo + 64, h, qa:qe],
                                           start=True, stop=True)
                    return ins
                S.op("tensor", smm, reads=[("Kt", kb_i, c["q4"]) for c in grp] + ["KPEs"], writes=[f"bank{c['sb']}" for c in grp])
                for c in grp:
                    S.op("scalar", lambda e, c=c, is_mla=is_mla: e.activation(
                        out=c["pt"], in_=c["sbank"], func=AF.Exp, scale=(sc_mla if is_mla else sc_diff)),
                        reads=[f"bank{c['sb']}"], writes=[c["ptk"]])
                    if c["qa"] == c["q0"]:
                        S.op("gpsimd", lambda e, c=c: e.tensor_tensor(
                            out=c["pt"][:, 0:128], in0=c["pt"][:, 0:128], in1=mask_sb[:, (c["kb"] // 8) % 2, c["kb"] % 8, :], op=ALU.mult),
                            reads=[c["ptk"], "mask"], writes=[c["ptk"]])
                    if c["kb"] == 0:
                        S.op("vector", lambda e, c=c, Psum=Psum: e.tensor_copy(out=Psum[:, c["qa"]:c["qe"]], in_=c["pt"]),
                             reads=[c["ptk"]], writes=[("Psum", pi_, c["half"])])
                    else:
                        S.op("vector", lambda e, c=c, Psum=Psum: e.tensor_tensor(out=Psum[:, c["qa"]:c["qe"]], in0=Psum[:, c["qa"]:c["qe"]],
                                                                                 in1=c["pt"], op=ALU.add),
                             reads=[c["ptk"], ("Psum", pi_, c["half"])], writes=[("Psum", pi_, c["half"])])
                nsc += 1
                warm_i[0] += 1
                if warm_i[0] % WARM_EVERY == 0:
                    def warm(e):
                        ins = None
                        for k in range(16):
                            ins = e.matmul(banks[7][:, 0:256], lhsT=ones_b[:], rhs=QN[:, k % 8, 0:256], start=(k == 0), stop=(k == 15))
                        return ins
                    S.op("tensor", warm, reads=["ones_b"], writes=["bank7"])
                if nsc == LOOK:
                    if dev[0] is not None:
                        while pend and pend[0][0] != hc_i:
                            pend.pop(0)[1]()
                        dev[0]()
                        dev[0] = None
                    if hh + 1 < 16 and comp == 0:
                        load_head(hh + 1)

                def emit_pv(grp=grp, Vt=Vt, kb_i=kb_i):
                    def pvmm(e):
                        ins = None
                        for c in grp:
                            c0 = c["qa"] % 512
                            ins = e.matmul(banks[c["obank"]][:, c0:c0 + c["n"]], lhsT=Vt[:, c["kb"], :], rhs=c["pt"],
                                           start=c["st"], stop=(c["kb"] == 63), skip_group_check=True)
                        return ins
                    S.op("tensor", pvmm, reads=[c["ptk"] for c in grp] + [("Vt", kb_i, c["q4"]) for c in grp],
                         writes=sorted({f"bank{c['obank']}" for c in grp}))
                pend.append((hc_i, emit_pv))
                if dev[0] is None:
                    while len(pend) > LOOK:
                        pend.pop(0)[1]()
            dev[0] = (lambda hh=hh, comp=comp, Psum=Psum, pi_=pi_: evac(hh, comp, Psum, pi_))
    while pend:
        pend.pop(0)[1]()
    dev[0]()
    if dbg:
        dbg_ops.append(S.op("sync", lambda e: e.dma_start(out=OCd.rearrange("(s p) d -> p s d", p=128), in_=OC), dma=True))
    S.barrier()
    if stop == "p2":
        run_sched(nc, S, dbg_ops)
        return nc
    A.off = 64 * KB

    wo = A.alloc([128, 16, D], BF16)
    wr = A.alloc([128, 16, 64], F32)
    gtgA_rep = A.alloc([128, D], F32)
    diagt = A.alloc([128, 128], F32)
    ocT = A.alloc([128, 16, 128], BF16)
    xo = A.alloc([128, D], F32)
    x1t = A.alloc([128, D], F32)
    xn_f = A.alloc([128, D], F32)
    yjunk = A.alloc([128, D], BF16)
    h2f = A.alloc([128, 16, 128], F32)
    sc_t = A.alloc([128, 64], F32)
    bi_t = A.alloc([128, 64], F32)
    mk_t = A.alloc([128, 64], F32)
    m8 = A.alloc([128, 8, 8], F32)
    gs_t = A.alloc([128, 8], F32)
    gm_t = A.alloc([128, 8], F32)
    pen_t = A.alloc([128, 8], F32)
    wjunk = A.alloc([128, 64], F32)
    S.op("gpsimd", lambda e: e.dma_start(out=wo, in_=w_out.rearrange("(kc p) n -> p kc n", p=128)), writes=["wo"], dma=True)
    S.op("sync", lambda e: e.dma_start(out=wr, in_=w_router.rearrange("(kc p) n -> p kc n", p=128)), writes=["wr"], dma=True)

    def build_rep(gcol, gkey, rep, repkey, diagt):
        for c in range(16):
            S.op("vector", lambda e, c=c: e.tensor_scalar(out=diagt, in0=ident_f[:], scalar1=gcol[:, c:c + 1], scalar2=None, op0=ALU.mult),
                 reads=["ident_f", gkey], writes=["diagt"])
            pb = 7
            po = banks[pb][:, 0:128]
            S.op("tensor", lambda e, po=po: e.matmul(po, lhsT=ones_f[:], rhs=diagt, start=True, stop=True),
                 reads=["diagt", "ones_f"], writes=[f"bank{pb}"])
            S.op("vector", lambda e, po=po, c=c: e.tensor_copy(out=rep[:, c * 128:(c + 1) * 128], in_=po),
                 reads=[f"bank{pb}"], writes=[repkey])
    build_rep(gtg_a, "gtg_a", gtgA_rep, "gtgA_rep", diagt)

    for s in range(8):
        S.op("sync", lambda e, s=s: e.dma_start(out=xo, in_=x_own[s * 128:(s + 1) * 128, :]), writes=["xo"], dma=True)
        ssp = [sm() for _ in range(4)]
        for cg in range(4):
            pb = 2 + cg
            po = banks[pb][:, :]
            mm_acc(po, [(OCT[:, kc, s * 128:(s + 1) * 128], wo[:, kc, cg * 512:(cg + 1) * 512]) for kc in range(16)],
                   reads=["wo"], writes=[f"bank{pb}"])
            S.op("scalar", lambda e, po=po, cg=cg, acc=ssp[cg][0]: e.activation(out=yjunk[:, cg * 512:(cg + 1) * 512], in_=po, func=AF.Square,
                                                                 accum_out=acc),
                 reads=[f"bank{pb}"], writes=[("yjunk", cg), ssp[cg][1]])
        ss, ssk = sm()
        rs, rsk = sm()
        S.op("vector", lambda e, ss=ss, a0=ssp[0][0], a1=ssp[1][0]: e.tensor_tensor(out=ss, in0=a0, in1=a1, op=ALU.add),
             reads=[ssp[0][1], ssp[1][1]], writes=[ssk])
        S.op("vector", lambda e, ss=ss, a2=ssp[2][0]: e.tensor_tensor(out=ss, in0=ss, in1=a2, op=ALU.add), reads=[ssk, ssp[2][1]], writes=[ssk])
        S.op("vector", lambda e, ss=ss, a3=ssp[3][0]: e.tensor_tensor(out=ss, in0=ss, in1=a3, op=ALU.add), reads=[ssk, ssp[3][1]], writes=[ssk])
        rstd_from_ss(ss, ssk, D, rs, rsk)
        for cg in range(4):
            pb = 2 + cg
            po = banks[pb][:, :]
            S.op("vector", lambda e, po=po, cg=cg, rs=rs: e.scalar_tensor_tensor(
                out=x1t[:, cg * 512:(cg + 1) * 512], in0=po, scalar=rs, in1=gtgA_rep[:, cg * 512:(cg + 1) * 512],
                op0=ALU.mult, op1=ALU.mult), reads=[f"bank{pb}", rsk, "gtgA_rep"], writes=[("x1t", cg)])
        S.op("gpsimd", lambda e: e.tensor_tensor(out=x1t, in0=x1t, in1=xo, op=ALU.add),
             reads=[("x1t", cg) for cg in range(4)] + ["xo"], writes=["x1t"])
        S.op("sync", lambda e, s=s: e.dma_start(out=X1[s * 128:(s + 1) * 128, :], in_=x1t), reads=["x1t"], writes=["X1"], dma=True)
        ss2, ss2k = sm()
        rs2, rs2k = sm()
        S.op("scalar", lambda e, ss2=ss2: e.activation(out=yjunk, in_=x1t, func=AF.Square, accum_out=ss2),
             reads=["x1t"], writes=[("yjunk", 0), ("yjunk", 1), ("yjunk", 2), ("yjunk", 3), ss2k])
        rstd_from_ss(ss2, ss2k, D, rs2, rs2k)
        S.op("vector", lambda e, rs2=rs2: e.tensor_scalar(out=xn_f, in0=x1t, scalar1=rs2, scalar2=None, op0=ALU.mult),
             reads=["x1t", rs2k], writes=["xn_f"])
        for q in range(4):
            pb = 2 + q
            pv = banks[pb][:, :].rearrange("p (a b) -> p a b", a=4)

            def trf(e, q=q, pv=pv):
                ins = None
                for j in range(4):
                    kc = q * 4 + j
                    ins = e.transpose(out=pv[:, j, :], in_=xn_f[:, kc * 128:(kc + 1) * 128], identity=ident_f[:])
                return ins
            S.op("tensor", trf, reads=["xn_f", "ident_f"], writes=[f"bank{pb}"])

            def evf(e, q=q, pv=pv):
                ins = None
                for j in range(4):
                    kc = q * 4 + j
                    ins = e.tensor_scalar(out=h2f[:, kc, :], in0=pv[:, j, :], scalar1=gsc_f[:, kc:kc + 1],
                                          scalar2=modc[:, 48 + kc:48 + kc + 1], op0=ALU.mult, op1=ALU.add)
                return ins
            S.op("vector", evf, reads=[f"bank{pb}", "gsc_f", "modc"], writes=[("h2f", q)])
            S.op("gpsimd", lambda e, q=q, s=s: e.tensor_copy(out=h2T[:, q * 4:(q + 1) * 4, s * 128:(s + 1) * 128], in_=h2f[:, q * 4:(q + 1) * 4, :]),
                 reads=[("h2f", q)], writes=[("h2T", s, q)])
        pl = banks[6][:, 0:64]
        mm_acc(pl, [(h2f[:, kc, :], wr[:, kc, :]) for kc in range(16)], reads=[("h2f", q) for q in range(4)] + ["wr"], writes=["bank6"])
        S.op("scalar", lambda e: e.activation(out=sc_t, in_=pl, func=AF.Sigmoid), reads=["bank6"], writes=["sc_t"])
        S.op("vector", lambda e: e.tensor_tensor(out=bi_t, in0=sc_t, in1=brt_sb[:], op=ALU.add), reads=["sc_t", "brt"], writes=["bi_t"])

        def gtop(e):
            ins = None
            for gi in range(8):
                ins = e.max(out=m8[:, gi, :], in_=bi_t[:, gi * 8:(gi + 1) * 8])
            return ins
        S.op("vector", gtop, reads=["bi_t"], writes=["m8"])
        S.op("vector", lambda e: e.tensor_tensor(out=gs_t, in0=m8[:, :, 0], in1=m8[:, :, 1], op=ALU.add), reads=["m8"], writes=["gs_t"])
        S.op("vector", lambda e: e.max(out=m8[:, 0, :], in_=gs_t), reads=["gs_t", "m8"], writes=["m8b"])
        S.op("vector", lambda e: e.tensor_scalar(out=gm_t, in0=gs_t, scalar1=m8[:, 0, 3:4], scalar2=None, op0=ALU.is_ge),
             reads=["gs_t", "m8b"], writes=["gm_t"])
        S.op("vector", lambda e: e.tensor_scalar(out=pen_t, in0=gm_t, scalar1=-1.0, scalar2=1e30, op0=ALU.add, op1=ALU.mult),
             reads=["gm_t"], writes=["pen_t"])

        def mk(e):
            ins = None
            for gi in range(8):
                ins = e.tensor_scalar(out=mk_t[:, gi * 8:(gi + 1) * 8], in0=bi_t[:, gi * 8:(gi + 1) * 8],
                                      scalar1=gm_t[:, gi:gi + 1], scalar2=pen_t[:, gi:gi + 1], op0=ALU.mult, op1=ALU.add)
            return ins
        S.op("vector", mk, reads=["bi_t", "gm_t", "pen_t"], writes=["mk_t"])
        S.op("vector", lambda e: e.max(out=m8[:, 1, :], in_=mk_t), reads=["mk_t", "m8b"], writes=["m8c"])
        S.op("vector", lambda e: e.tensor_scalar(out=mk_t, in0=mk_t, scalar1=m8[:, 1, 7:8], scalar2=None, op0=ALU.is_ge),
             reads=["mk_t", "m8c"], writes=["mk_t"])
        ws_, wsk = sm()
        S.op("vector", lambda e, s=s: e.tensor_tensor(out=GATE[:, s, :], in0=mk_t, in1=sc_t, op=ALU.mult),
             reads=["mk_t", "sc_t"], writes=[("GATE", s)])
        S.op("vector", lambda e, s=s, ws_=ws_: e.reduce_sum(out=ws_, in_=GATE[:, s, :], axis=mybir.AxisListType.X),
             reads=[("GATE", s)], writes=[wsk])
        S.op("vector", lambda e, ws_=ws_: e.reciprocal(out=ws_, in_=ws_), reads=[wsk], writes=[wsk])
        S.op("vector", lambda e, s=s, ws_=ws_: e.tensor_scalar(out=GATE[:, s, :], in0=GATE[:, s, :], scalar1=ws_, scalar2=2.5,
                                                              op0=ALU.mult, op1=ALU.mult), reads=[("GATE", s), wsk], writes=[("GATE", s)])
    if dbg:
        dbg_ops.append(S.op("sync", lambda e: e.dma_start(out=GATEd, in_=GATE[:]), reads=[("GATE", s) for s in range(8)], dma=True))
    if stop == "p3":
        S.barrier()
        run_sched(nc, S, dbg_ops)
        return nc
    h2T_keys = [("h2T", s, q) for s in range(8) for q in range(4)]
    S.barrier()
    A.off = 32 * KB
    GT = A.alloc([64, NOWN], F32)
    Yacc = A.alloc([128, 8, D], F32)
    AT = A.alloc([128, 4, NOWN], BF16)
    Wg = [A.alloc([128, 16, 512], BF16) for _ in range(2)]
    Wu = [A.alloc([128, 16, 512], BF16) for _ in range(2)]
    Wd = [A.alloc([128, 4, D], BF16) for _ in range(1)]
    sg_t = [A.alloc([128, 512], F32) for _ in range(2)]
    grep = A.alloc([128, NOWN], F32)
    for s in range(8):
        pb = 7
        po = banks[pb][0:64, 0:128]
        S.op("tensor", lambda e, po=po, s=s: e.transpose(out=po, in_=GATE[:, s, :], identity=ident_f[:]),
             reads=["ident_f"], writes=[f"bank{pb}"])
        S.op("vector", lambda e, po=po, s=s: e.tensor_copy(out=GT[:, s * 128:(s + 1) * 128], in_=po), reads=[f"bank{pb}"], writes=["GT"])

    for ei in range(NEXP + 1):
        b = ei % 2
        if ei < NEXP:
            gsrc, usrc, dsrc = w_gate[ei], w_up[ei], w_down[ei]
        else:
            gsrc, usrc, dsrc = ws_gate, ws_up, ws_down
        for q in range(2):
            S.op("gpsimd", lambda e, b=b, gsrc=gsrc, q=q: e.dma_start(
                out=Wg[b][:, q * 8:(q + 1) * 8, :], in_=gsrc.rearrange("(kc p) n -> p kc n", p=128)[:, q * 8:(q + 1) * 8, :]),
                writes=[("Wg", b, q)], dma=True)
            S.op("gpsimd", lambda e, b=b, usrc=usrc, q=q: e.dma_start(
                out=Wu[b][:, q * 8:(q + 1) * 8, :], in_=usrc.rearrange("(kc p) n -> p kc n", p=128)[:, q * 8:(q + 1) * 8, :]),
                writes=[("Wu", b, q)], dma=True)
        S.op("gpsimd", lambda e, dsrc=dsrc: e.dma_start(out=Wd[0], in_=dsrc.rearrange("(kc p) n -> p kc n", p=128)),
             writes=["Wd"], dma=True)
        if ei < NEXP:
            for half in range(2):
                pb = 7
                po = banks[pb][:, :]
                S.op("tensor", lambda e, po=po, ei=ei, half=half: e.matmul(
                    po, lhsT=ident_f[0:64, ei:ei + 1].broadcast_to([64, 128]), rhs=GT[:, half * 512:(half + 1) * 512],
                    start=True, stop=True), reads=["GT", "ident_f"], writes=[f"bank{pb}"])
                S.op("vector", lambda e, po=po, half=half: e.tensor_copy(out=grep[:, half * 512:(half + 1) * 512], in_=po),
                     reads=[f"bank{pb}"], writes=[("grep", half)])
        for half in range(2):
            for hc in range(4):
                pg = next_bank([0, 1])
                pu = next_bank([2, 3])
                mm_acc(banks[pg][:, :], [(Wg[b][:, kc, hc * 128:(hc + 1) * 128], h2T[:, kc, half * 512:(half + 1) * 512]) for kc in range(16)],
                       reads=[("Wg", b, 0), ("Wg", b, 1)], writes=[f"bank{pg}"])
                mm_acc(banks[pu][:, :], [(Wu[b][:, kc, hc * 128:(hc + 1) * 128], h2T[:, kc, half * 512:(half + 1) * 512]) for kc in range(16)],
                       reads=[("Wu", b, 0), ("Wu", b, 1)], writes=[f"bank{pu}"])
                sgi = (half * 4 + hc) % 2
                sg = sg_t[sgi]
                S.op("scalar", lambda e, pg=pg, sg=sg: e.activation(out=sg, in_=banks[pg][:, :], func=AF.Silu),
                     reads=[f"bank{pg}"], writes=[("sg", sgi)])
                if ei < NEXP:
                    S.op("gpsimd", lambda e, sg=sg, half=half: e.tensor_tensor(out=sg, in0=sg, in1=grep[:, half * 512:(half + 1) * 512], op=ALU.mult),
                         reads=[("sg", sgi), ("grep", half)], writes=[("sg", sgi)])
                S.op("vector", lambda e, pu=pu, sg=sg, hc=hc, half=half: e.tensor_tensor(
                    out=AT[:, hc, half * 512:(half + 1) * 512], in0=banks[pu][:, :], in1=sg, op=ALU.mult),
                    reads=[f"bank{pu}", ("sg", sgi)], writes=[("AT", hc, half)])
        for tile in range(8):
            for cg in range(4):
                pb = next_bank([4, 5, 6])
                po = banks[pb][:, :]
                mm_acc(po, [(AT[:, hc, tile * 128:(tile + 1) * 128], Wd[0][:, hc, cg * 512:(cg + 1) * 512]) for hc in range(4)],
                       reads=[("AT", hc, tile // 4) for hc in range(4)] + ["Wd"], writes=[f"bank{pb}"])
                if ei == 0:
                    S.op("vector", lambda e, po=po, tile=tile, cg=cg: e.tensor_copy(out=Yacc[:, tile, cg * 512:(cg + 1) * 512], in_=po),
                         reads=[f"bank{pb}"], writes=[("Y", tile, cg)])
                else:
                    S.op("vector", lambda e, po=po, tile=tile, cg=cg: e.tensor_tensor(
                        out=Yacc[:, tile, cg * 512:(cg + 1) * 512], in0=po, in1=Yacc[:, tile, cg * 512:(cg + 1) * 512], op=ALU.add),
                        reads=[f"bank{pb}", ("Y", tile, cg)], writes=[("Y", tile, cg)])
    S.barrier()

    A.off = 32 * KB
    _gt = A.alloc([64, NOWN], F32)
    _y = A.alloc([128, 8, D], F32)
    gtgF_rep = A.alloc([128, D], F32)
    diagt2 = A.alloc([128, 128], F32)
    x1b = [A.alloc([128, D], F32) for _ in range(8)]
    fjunk = [A.alloc([128, D], BF16) for _ in range(2)]
    build_rep(gtg_f, "gtg_f", gtgF_rep, "gtgF_rep", diagt2)
    finals = []
    for tile in range(8):
        S.op("sync", lambda e, xb=x1b[tile], tile=tile: e.dma_start(out=xb, in_=X1[tile * 128:(tile + 1) * 128, :]),
             writes=[("x1b", tile)], dma=True)
    for tile in range(8):
        xb = x1b[tile]
        xk = ("x1b", tile)
        ss, ssk = sm()
        rs, rsk = sm()
        S.op("scalar", lambda e, tile=tile, ss=ss: e.activation(out=fjunk[tile % 2], in_=Yacc[:, tile, :], func=AF.Square, accum_out=ss),
             writes=[("fjunk", tile % 2), ssk])
        rstd_from_ss(ss, ssk, D, rs, rsk)
        S.op("vector", lambda e, tile=tile, rs=rs: e.scalar_tensor_tensor(out=Yacc[:, tile, :], in0=Yacc[:, tile, :], scalar=rs,
                                                                           in1=gtgF_rep, op0=ALU.mult, op1=ALU.mult),
             reads=[rsk, "gtgF_rep"], writes=[("Yf", tile)])
        S.op("gpsimd", lambda e, tile=tile, xb=xb: e.tensor_tensor(out=xb, in0=xb, in1=Yacc[:, tile, :], op=ALU.add),
             reads=[("Yf", tile), xk], writes=[xk])
        finals.append(S.op("sync", lambda e, tile=tile, xb=xb: e.dma_start(out=out[tile * 128:(tile + 1) * 128, :], in_=xb),
                           reads=[xk], writes=[("out", tile)], dma=True))
    run_sched(nc, S, finals + dbg_ops)
    return nc


def _slot_blocks(c):
    return [8 * s + c if s % 2 == 0 else 8 * s + 7 - c for s in range(8)]


def _rope_tables():
    pos = np.arange(SEQ, dtype=np.float32)
    inv_k = (np.float32(THETA) ** (-np.arange(0, 64, 2, dtype=np.float32) / np.float32(64))).astype(np.float32)
    ang = (pos[:, None] * inv_k[None, :]).astype(np.float32)
    cos, sin = np.cos(ang).astype(np.float32), np.sin(ang).astype(np.float32)
    ck = np.concatenate([cos, cos], axis=1).T
    sk = np.concatenate([-sin, sin], axis=1).T
    ropeK = np.ascontiguousarray(np.stack([ck, sk]).astype(np.float32))
    inv_d = (np.float32(THETA) ** (-np.arange(0, 16, 2, dtype=np.float32) / np.float32(16))).astype(np.float32)
    angd = (pos[:, None] * inv_d[None, :]).astype(np.float32)
    cd, sd = np.cos(angd).astype(np.float32), np.sin(angd).astype(np.float32)
    c64 = np.ones((SEQ, 64), np.float32)
    s64 = np.zeros((SEQ, 64), np.float32)
    c64[:, 0:8] = cd
    c64[:, 8:16] = cd
    s64[:, 0:8] = -sd
    s64[:, 8:16] = sd
    cD = np.concatenate([c64, c64], axis=1).T
    sD = np.concatenate([s64, s64], axis=1).T
    ropeD = np.ascontiguousarray(np.stack([cD, sD]).astype(np.float32))
    return ropeK, ropeD


def _perms():
    P = np.zeros((128, 192), np.float32)
    for m in range(128):
        d = m % 64
        if d < 8:
            P[m + 8, m] = 1.0
        elif d < 16:
            P[m - 8, m] = 1.0
    for m in range(64):
        P[(m + 32) % 64, 128 + m] = 1.0
    return P.astype(ml_dtypes.bfloat16)


def _masks(c):
    diag = np.ones((128, 128), np.float32)
    diag[64:, :64] = 0.0
    M = np.zeros((128, 2, 8, 128), np.float32)
    for par in range(2):
        jd = c if par == 0 else 7 - c
        for j in range(8):
            if j < jd:
                M[:, par, j, :] = 1.0
            elif j == jd:
                M[:, par, j, :] = diag
    return M.astype(ml_dtypes.bfloat16)


_NC_CACHE = {}


def _col(v, n):
    return np.ascontiguousarray(np.asarray(v, np.float32).reshape(n, 128).T)


def make_in_maps(inputs):
    f = lambda k: np.asarray(inputs[k], np.float32)
    x = f("x")[0]
    ropeK, ropeD = _rope_tables()
    perms = _perms()
    shared = {
        "x_all": x,
        "c_col": _col(f("c")[0], 16),
        "w_ada": f("w_ada")[0],
        "b_ada_col": _col(f("b_ada")[0], 96),
        "gcols": np.ascontiguousarray(np.concatenate([_col(f(k)[0], 16) for k in ("g_pre_mix", "g_post_mix", "g_pre_ffn", "g_post_ffn")], axis=1)),
        "gq_col": _col(f("g_q_lat")[0], 6),
        "gkv_col": _col(f("g_kv_lat")[0], 4),
        "lam_row": np.ascontiguousarray(np.concatenate([f(k)[0] for k in ("lambda_q1", "lambda_k1", "lambda_q2", "lambda_k2")])[None, :]),
        "gsub_row": f("g_diff_sub"),
        "gsub_col": np.ascontiguousarray(f("g_diff_sub")[0][:, None]),
        "brt_row": f("b_router"),
        "w_in": f("w_in")[0], "w_uq": f("w_uq")[0], "w_ukv": f("w_ukv")[0], "w_out": f("w_out")[0],
        "w_router": f("w_router")[0], "w_gate": f("w_gate")[0], "w_up": f("w_up")[0], "w_down": f("w_down")[0],
        "ws_gate": f("ws_gate")[0], "ws_up": f("ws_up")[0], "ws_down": f("ws_down")[0],
        "ropeK": ropeK, "ropeD": ropeD, "perms": perms,
    }
    maps = []
    for c in range(NCORES):
        rows = np.concatenate([np.arange(b * 128, (b + 1) * 128) for b in _slot_blocks(c)])
        m = dict(shared)
        m["x_own"] = np.ascontiguousarray(x[rows])
        m["ropeKq"] = np.ascontiguousarray(ropeK[:, :, rows])
        m["ropeDq"] = np.ascontiguousarray(ropeD[:, :, rows])
        m["masks"] = _masks(c)
        maps.append(m)
    return maps


def kernel(**inputs):
    if "nc" not in _NC_CACHE:
        _NC_CACHE["nc"] = build_program(False)
    nc = _NC_CACHE["nc"]
    maps = make_in_maps(inputs)
    res = run_bass_kernel_spmd(nc, maps, core_ids=list(range(NCORES)))
    outp = np.zeros((1, SEQ, D), np.float32)
    for c in range(NCORES):
        o = np.asarray(res.results[c]["out"], np.float32)
        for s, b in enumerate(_slot_blocks(c)):
            outp[0, b * 128:(b + 1) * 128, :] = o[s * 128:(s + 1) * 128, :]
    return outp
```
